# Optimizing a Trainium2 kernel written in Bass

```python
import math
import jax, jax.numpy as jnp
from jax import lax
import numpy as np

D_MODEL = 1024
BATCH = 4
SEQ = 4096
DEPTH = 2

CHUNK = 64
Q_BLOCK = 128
EPS = 1e-6
MAX_OFFSET = 4096

HG_HEADS = 4
HG_DK = 128
HG_DV = 128
HG_WIDTH = HG_HEADS * HG_DV

MLA_HEADS = 8
MLA_NOPE = 64
MLA_ROPE = 32
MLA_V = 64
MLA_Q_LORA = 384
MLA_KV_LORA = 256
MLA_DQK = MLA_NOPE + MLA_ROPE
MLA_WIDTH = MLA_HEADS * MLA_V
ROPE_BASE = 10000.0

N_EXPERTS = 16
N_GROUPS = 4
EXPERTS_PER_GROUP = N_EXPERTS // N_GROUPS
TOPK_GROUPS = 1
TOP_K = 2
D_EXPERT = 512

IN_SIZES = (HG_HEADS * HG_DK,
            HG_HEADS * HG_DK,
            HG_HEADS * HG_DV,
            HG_WIDTH,
            MLA_Q_LORA,
            MLA_KV_LORA,
            MLA_ROPE,
            2 * D_MODEL)
IN_COLS = int(sum(IN_SIZES))
IN_SPLITS = tuple(int(v) for v in np.cumsum(IN_SIZES)[:-1])

kernel_name = "hybrid_hgrn2_mla_grouped_moe_adaln"


def rmsnorm(x, g):
    xf = x.astype(jnp.float32)
    y = xf * lax.rsqrt(jnp.mean(xf * xf, axis=-1, keepdims=True) + EPS)
    return (y * g.astype(jnp.float32)).astype(x.dtype)


def rotary(x, pos):
    half = x.shape[-1] // 2
    inv = ROPE_BASE ** (-jnp.arange(half, dtype=jnp.float32) / half)
    ang = pos.astype(jnp.float32)[..., None] * inv
    cos = jnp.cos(ang)[:, :, None, :]
    sin = jnp.sin(ang)[:, :, None, :]
    xf = x.astype(jnp.float32)
    x1, x2 = xf[..., :half], xf[..., half:]
    return jnp.concatenate([x1 * cos - x2 * sin, x1 * sin + x2 * cos], -1).astype(x.dtype)


def hgrn2_mix(q, f_logit, v, lb):
    B, S = q.shape[0], q.shape[1]
    nc = S // CHUNK
    lbh = lb.astype(jnp.float32).reshape(HG_HEADS, HG_DK)
    f = lbh + (1.0 - lbh) * jax.nn.sigmoid(f_logit.astype(jnp.float32))
    log_f = jnp.log(f)
    k = 1.0 - f
    qf = jax.nn.silu(q.astype(jnp.float32)) * (HG_DK ** -0.5)
    vf = v.astype(jnp.float32)

    def to_chunks(t):
        return t.reshape(B, nc, CHUNK, HG_HEADS, t.shape[-1]).transpose(1, 0, 3, 2, 4)

    qc, kc, vc, gc = to_chunks(qf), to_chunks(k), to_chunks(vf), to_chunks(log_f)
    causal = jnp.tril(jnp.ones((CHUNK, CHUNK), dtype=bool))[:, :, None]

    def step(state, inp):
        qb, kb, vb, gb = inp
        b = jnp.cumsum(gb, axis=2)
        rel = b[:, :, :, None, :] - b[:, :, None, :, :]
        decay = jnp.exp(jnp.where(causal, rel, -jnp.inf))
        attn = jnp.einsum('bhtd,bhtsd,bhsd->bhts', qb, decay, kb)
        intra = jnp.einsum('bhts,bhsv->bhtv', attn, vb)
        inter = jnp.einsum('bhtd,bhdv->bhtv', qb * jnp.exp(b), state)
        b_last = b[:, :, -1:, :]
        k_dec = kb * jnp.exp(b_last - b)
        new_state = state * jnp.exp(b_last[:, :, 0, :, None]) + jnp.einsum('bhsd,bhsv->bhdv', k_dec, vb)
        return new_state, intra + inter

    state0 = jnp.zeros((B, HG_HEADS, HG_DK, HG_DV), jnp.float32)
    _, out = lax.scan(step, state0, (qc, kc, vc, gc))
    return out.transpose(1, 0, 3, 2, 4).reshape(B, S, HG_HEADS, HG_DV)


def mla_mix(c_q, c_kv, k_rope, pos, q_norm_g, w_q_up, kv_norm_g, w_kv_up):
    B, S = c_q.shape[0], c_q.shape[1]
    q = (rmsnorm(c_q, q_norm_g) @ w_q_up).reshape(B, S, MLA_HEADS, MLA_DQK)
    q = jnp.concatenate([q[..., :MLA_NOPE], rotary(q[..., MLA_NOPE:], pos)], -1)
    kv = (rmsnorm(c_kv, kv_norm_g) @ w_kv_up).reshape(B, S, MLA_HEADS, MLA_NOPE + MLA_V)
    k_nope, v = kv[..., :MLA_NOPE], kv[..., MLA_NOPE:]
    k_pe = rotary(k_rope[:, :, None, :], pos)
    k = jnp.concatenate([k_nope, jnp.broadcast_to(k_pe, (B, S, MLA_HEADS, MLA_ROPE)).astype(k_nope.dtype)], -1)
    scale = MLA_DQK ** -0.5
    nb = S // Q_BLOCK
    qb = q.reshape(B, nb, Q_BLOCK, MLA_HEADS, MLA_DQK).transpose(1, 0, 2, 3, 4)
    key_chunk = jnp.arange(S) // CHUNK

    def block(args):
        qblk, start = args
        q_chunk = (start + jnp.arange(Q_BLOCK)) // CHUNK
        s = jnp.einsum('bqhd,bkhd->bhqk', qblk, k, preferred_element_type=jnp.float32) * scale
        mask = key_chunk[None, :] <= q_chunk[:, None]
        p = jax.nn.softmax(jnp.where(mask, s, -jnp.inf), axis=-1).astype(v.dtype)
        return jnp.einsum('bhqk,bkhd->bqhd', p, v)

    out = lax.map(block, (qb, jnp.arange(nb, dtype=jnp.int32) * Q_BLOCK))
    return out.transpose(1, 0, 2, 3, 4).reshape(B, S, MLA_WIDTH)


def token_mix(h, pos, w_in, lb, hg_norm_g, q_norm_g, w_q_up, kv_norm_g, w_kv_up, w_br_a, w_br_b, w_out):
    B, S, D = h.shape
    proj = h @ w_in
    hq, hf, hi, hgate, cq, ckv, kr, gates = jnp.split(proj, IN_SPLITS, axis=-1)
    o_a = hgrn2_mix(hq.reshape(B, S, HG_HEADS, HG_DK), hf.reshape(B, S, HG_HEADS, HG_DK),
                    hi.reshape(B, S, HG_HEADS, HG_DV), lb)
    o_a = rmsnorm(o_a, hg_norm_g.reshape(HG_HEADS, HG_DV)).reshape(B, S, HG_WIDTH)
    o_a = (o_a * jax.nn.silu(hgate.astype(jnp.float32))).astype(h.dtype)
    y_a = o_a @ w_br_a
    y_b = mla_mix(cq, ckv, kr, pos, q_norm_g, w_q_up, kv_norm_g, w_kv_up) @ w_br_b
    g = jax.nn.sigmoid(gates.astype(jnp.float32))
    g_a, g_b = g[..., :D], g[..., D:]
    merged = (g_a * y_a.astype(jnp.float32) + g_b * y_b.astype(jnp.float32)).astype(h.dtype)
    return merged @ w_out


def grouped_moe(h, w_router, router_bias, w_gate, w_up, w_down):
    B, S, D = h.shape
    t = h.reshape(-1, D)
    scores = jax.nn.sigmoid((t @ w_router).astype(jnp.float32))
    biased = (scores + router_bias.astype(jnp.float32)).reshape(-1, N_GROUPS, EXPERTS_PER_GROUP)
    group_score = lax.top_k(biased, 2)[0].sum(-1)
    _, gidx = lax.top_k(group_score, TOPK_GROUPS)
    gmask = jax.nn.one_hot(gidx, N_GROUPS, dtype=jnp.float32).sum(-2) > 0
    masked = jnp.where(gmask[:, :, None], biased, -jnp.inf).reshape(-1, N_EXPERTS)
    _, eidx = lax.top_k(masked, TOP_K)
    sel = jnp.take_along_axis(scores, eidx, axis=-1)
    w = sel / sel.sum(-1, keepdims=True)
    combine = (jax.nn.one_hot(eidx, N_EXPERTS, dtype=jnp.float32) * w[..., None]).sum(-2)
    y = jnp.zeros(t.shape, jnp.float32)
    for e in range(N_EXPERTS):
        he = jax.nn.silu(t @ w_gate[e]) * (t @ w_up[e])
        y = y + combine[:, e:e + 1] * (he @ w_down[e]).astype(jnp.float32)
    return y.astype(h.dtype).reshape(B, S, D)


def setup_inputs(seed: int = 0) -> dict:
    key = jax.random.key(seed)
    ks = jax.random.split(key, 24)
    f32 = jnp.float32
    D = D_MODEL

    def nrm(k, shape, scale):
        return jax.random.normal(k, shape, f32) * scale

    positions = (jax.random.randint(ks[2], (BATCH, 1), 0, MAX_OFFSET, dtype=jnp.int32)
                 + jnp.arange(SEQ, dtype=jnp.int32)[None, :]).astype(jnp.int32)
    return {
        "x": nrm(ks[0], (BATCH, SEQ, D), 1.0),
        "c": nrm(ks[1], (BATCH, D), 1.0),
        "positions": positions,
        "ada_w": nrm(ks[3], (DEPTH, D, 6 * D), 0.5 * D ** -0.5),
        "ada_b": nrm(ks[4], (DEPTH, 6 * D), 0.02),
        "norm1_g": 1.0 + nrm(ks[5], (DEPTH, D), 0.02),
        "w_in": nrm(ks[6], (DEPTH, D, IN_COLS), D ** -0.5),
        "hg_lb_logits": nrm(ks[7], (DEPTH, HG_HEADS * HG_DK), 1.0),
        "hg_norm_g": 1.0 + nrm(ks[8], (DEPTH, HG_WIDTH), 0.02),
        "q_norm_g": 1.0 + nrm(ks[9], (DEPTH, MLA_Q_LORA), 0.02),
        "w_q_up": nrm(ks[10], (DEPTH, MLA_Q_LORA, MLA_HEADS * MLA_DQK), MLA_Q_LORA ** -0.5),
        "kv_norm_g": 1.0 + nrm(ks[11], (DEPTH, MLA_KV_LORA), 0.02),
        "w_kv_up": nrm(ks[12], (DEPTH, MLA_KV_LORA, MLA_HEADS * (MLA_NOPE + MLA_V)), MLA_KV_LORA ** -0.5),
        "w_br_a": nrm(ks[13], (DEPTH, HG_WIDTH, D), HG_WIDTH ** -0.5),
        "w_br_b": nrm(ks[14], (DEPTH, MLA_WIDTH, D), MLA_WIDTH ** -0.5),
        "w_out": nrm(ks[15], (DEPTH, D, D), D ** -0.5),
        "norm2_g": 1.0 + nrm(ks[16], (DEPTH, D), 0.02),
        "w_router": nrm(ks[17], (D, N_EXPERTS), D ** -0.5),
        "router_bias": nrm(ks[18], (N_EXPERTS,), 0.01),
        "w_gate": nrm(ks[19], (DEPTH, N_EXPERTS, D, D_EXPERT), D ** -0.5),
        "w_up": nrm(ks[20], (DEPTH, N_EXPERTS, D, D_EXPERT), D ** -0.5),
        "w_down": nrm(ks[21], (DEPTH, N_EXPERTS, D_EXPERT, D), D_EXPERT ** -0.5),
        "final_g": 1.0 + nrm(ks[22], (D,), 0.02),
    }


def reference(x, c, positions, ada_w, ada_b, norm1_g, w_in, hg_lb_logits, hg_norm_g,
              q_norm_g, w_q_up, kv_norm_g, w_kv_up, w_br_a, w_br_b, w_out, norm2_g,
              w_router, router_bias, w_gate, w_up, w_down, final_g):
    D = D_MODEL
    lb_soft = jax.nn.softmax(hg_lb_logits.astype(jnp.float32), axis=0)
    lb_all = jnp.cumsum(lb_soft, axis=0) - lb_soft[0:1]
    c_act = jax.nn.silu(c)
    for l in range(DEPTH):
        mod = c_act @ ada_w[l] + ada_b[l]
        sh1, sc1, g1 = mod[:, None, 0:D], mod[:, None, D:2 * D], mod[:, None, 2 * D:3 * D]
        sh2, sc2, g2 = mod[:, None, 3 * D:4 * D], mod[:, None, 4 * D:5 * D], mod[:, None, 5 * D:6 * D]
        h = rmsnorm(x, norm1_g[l]) * (1.0 + sc1) + sh1
        x = x + g1 * token_mix(h, positions, w_in[l], lb_all[l], hg_norm_g[l], q_norm_g[l], w_q_up[l],
                               kv_norm_g[l], w_kv_up[l], w_br_a[l], w_br_b[l], w_out[l])
        h = rmsnorm(x, norm2_g[l]) * (1.0 + sc2) + sh2
        x = x + g2 * grouped_moe(h, w_router, router_bias, w_gate[l], w_up[l], w_down[l])
    return rmsnorm(x, final_g)
```

```python
import numpy as np
from contextlib import ExitStack
import concourse.bass as bass
import concourse.mybir as mybir
from concourse.bass_utils import run_bass_kernel_spmd

F32 = mybir.dt.float32
F32R = mybir.dt.float32r
I32 = mybir.dt.int32
ALU = mybir.AluOpType
AF = mybir.ActivationFunctionType
AX = mybir.AxisListType

ENGS = ("pe", "act", "dve", "pool", "sp")

D = 1024
DEPTH = 2
HG_H = 4
MLA_H = 8
NEXP = 16
EPS = 1e-6
ROPE_BASE = 10000.0
TWO_PI = 2.0 * np.pi


class V:
    def __init__(self, ap, tok):
        self.ap = ap
        self.tok = tok

    def __getitem__(self, k):
        return V(self.ap[k], self.tok)

    def bc(self, dt):
        return V(self.ap.bitcast(dt), self.tok)

    def re(self, s, **kw):
        return V(self.ap.rearrange(s, **kw), self.tok)

    def bcast(self, shape):
        return V(self.ap.to_broadcast(list(shape)), self.tok)

    def bto(self, shape):
        return V(self.ap.broadcast_to(list(shape)), self.tok)

    def sub(self, tok):
        return V(self.ap, tok)


class Prog:
    def __init__(self, nc, ndma=32):
        self.nc = nc
        self.es = ExitStack()
        self.scopes = []
        self.stream = {e: [] for e in ENGS}
        self.know = {e: {} for e in ENGS}
        self.tok = {}
        self.esem = {e: self.es.enter_context(nc.semaphore("s_" + e)) for e in ENGS}
        self.ndma = ndma
        self.dsem = [self.es.enter_context(nc.semaphore("d%d" % i)) for i in range(ndma + 1)]
        self.dcnt = [0] * (ndma + 1)
        self.dsnap = {}
        self.dn = 0
        self.uid = 0
        self.dry = False

    def push(self):
        self.scopes.append(ExitStack())

    def pop(self):
        self.scopes.pop().close()
        self.barrier()

    def _ctx(self):
        return self.scopes[-1] if self.scopes else self.es

    def sb(self, name, shape, dt=F32):
        self.uid += 1
        nm = "%s_%d" % (name, self.uid)
        t = self._ctx().enter_context(self.nc.sbuf_tensor(nm, list(shape), dt))
        return V(t[:], nm)

    def ps(self, name, shape, dt=F32):
        t = self.es.enter_context(self.nc.psum_tensor(name, list(shape), dt))
        return V(t[:], name)

    def _deps(self, reads, writes):
        deps = set()
        for t in reads:
            st = self.tok.get(t)
            if st and st[0] is not None:
                deps.add(st[0])
        for t in writes:
            st = self.tok.get(t)
            if st:
                if st[0] is not None:
                    deps.add(st[0])
                deps.update(st[1])
        return deps

    def _snap_of(self, ev):
        if ev[0] == "e":
            return self.stream[ev[1]][ev[2]]["snap"]
        return self.dsnap[(ev[1], ev[2])]

    def _resolve(self, eng, deps, is_dma):
        kn = self.know[eng]
        best = {}
        for ev in deps:
            key = (ev[0], ev[1])
            if best.get(key, (-1,))[0] < ev[2]:
                best[key] = (ev[2], ev)
        waits = []
        for key, (val, ev) in best.items():
            if key == ("e", eng) and eng == "pe":
                continue
            if kn.get(key, -1) >= val:
                continue
            if ev[0] == "e":
                self.stream[ev[1]][ev[2]]["inc"] = True
            waits.append(ev)
            snap = self._snap_of(ev)
            for k2, v2 in snap.items():
                if kn.get(k2, -1) < v2:
                    kn[k2] = v2
            if kn.get(key, -1) < val:
                kn[key] = val
        return waits

    def _register(self, ev, reads, writes):
        for t in reads:
            st = self.tok.get(t)
            if st is None:
                st = [None, []]
                self.tok[t] = st
            st[1].append(ev)
        for t in writes:
            self.tok[t] = [ev, []]

    def op(self, eng, fn, reads=(), writes=()):
        if self.dry:
            return None
        deps = self._deps(reads, writes)
        waits = self._resolve(eng, deps, False)
        seq = len(self.stream[eng])
        snap = dict(self.know[eng])
        snap[("e", eng)] = seq
        self.stream[eng].append({"fn": fn, "waits": waits, "inc": False, "dma": None, "snap": snap})
        ev = ("e", eng, seq)
        self._register(ev, reads, writes)
        return ev

    def dma(self, q, out, in_, reads=(), writes=(), **kw):
        if self.dry:
            return None
        reads, writes = [in_.tok] + list(reads), [out.tok] + list(writes)
        k = self.dn % self.ndma
        self.dn += 1
        deps = self._deps(reads, writes)
        if self.dcnt[k] > 0:
            deps.add(("d", k, self.dcnt[k]))
        waits = self._resolve(q, deps, True)
        self.dcnt[k] += 16
        val = self.dcnt[k]

        def fn(eng, o=out.ap, i=in_.ap, kw=kw):
            return eng.dma_start(out=o, in_=i, **kw)

        self.stream[q].append({"fn": fn, "waits": waits, "inc": False, "dma": k,
                               "snap": dict(self.know[q])})
        self.dsnap[(k, val)] = dict(self.know[q])
        ev = ("d", k, val)
        self._register(ev, reads, writes)
        return ev

    def barrier(self):
        last = {}
        for e in ENGS:
            st = self.stream[e]
            for i in range(len(st) - 1, -1, -1):
                if st[i]["fn"] is not None and st[i]["dma"] is None:
                    last[e] = ("e", e, i)
                    break
        deps = set(last.values())
        for k in range(self.ndma + 1):
            if self.dcnt[k] > 0:
                deps.add(("d", k, self.dcnt[k]))
        for e in ENGS:
            waits = self._resolve(e, deps, True)
            self.stream[e].append({"fn": None, "waits": waits, "inc": False, "dma": None,
                                   "snap": dict(self.know[e])})
        self.tok = {}

    def emit(self):
        nc = self.nc
        pref = {}
        for e in ENGS:
            c = 0
            arr = []
            for ent in self.stream[e]:
                if ent["inc"]:
                    c += 1
                arr.append(c)
            pref[e] = arr

        def replay(e, eng):
            for ent in self.stream[e]:
                for ev in ent["waits"]:
                    if ev[0] == "e":
                        eng.wait_ge(self.esem[ev[1]], pref[ev[1]][ev[2]])
                    else:
                        eng.wait_ge(self.dsem[ev[1]], ev[2])
                if ent["fn"] is None:
                    continue
                ins = ent["fn"](eng)
                if ent["dma"] is not None:
                    ins.then_inc(self.dsem[ent["dma"]], ent.get("dinc", 16))
                elif ent["inc"]:
                    ins.then_inc(self.esem[e], 1)

        with nc.Block() as block:
            @block.tensor
            def _(eng):
                replay("pe", eng)

            @block.scalar
            def _(eng):
                replay("act", eng)

            @block.vector
            def _(eng):
                replay("dve", eng)

            @block.gpsimd
            def _(eng):
                replay("pool", eng)

            @block.sync
            def _(eng):
                replay("sp", eng)

    def close(self):
        while self.scopes:
            self.pop()
        self.es.close()

    @staticmethod
    def _rw(outs, ins):
        w = [o.tok for o in outs if isinstance(o, V)]
        r = [i.tok for i in ins if isinstance(i, V)]
        return r, w

    @staticmethod
    def _a(x):
        return x.ap if isinstance(x, V) else x

    def mm(self, out, lhsT, rhs, start=True, stop=True):
        r, w = self._rw([out], [lhsT, rhs])
        if not start:
            r = r + [out.tok]
        self.op("pe", lambda e, o=out.ap, l=lhsT.ap, rr=rhs.ap: e.matmul(o, lhsT=l, rhs=rr, start=start, stop=stop), r, w)

    def tr(self, out, in_, ident):
        r, w = self._rw([out], [in_, ident])
        self.op("pe", lambda e, o=out.ap, i=in_.ap, d=ident.ap: e.transpose(out=o, in_=i, identity=d), r, w)

    def act(self, out, in_, func, scale=None, bias=None, accum=None, extra_w=()):
        r, w = self._rw([out, accum], [in_, scale, bias])
        kw = {}
        if scale is not None:
            kw["scale"] = self._a(scale)
        if bias is not None:
            kw["bias"] = self._a(bias)
        if accum is not None:
            kw["accum_out"] = accum.ap
        self.op("act", lambda e, o=out.ap, i=in_.ap, kw=kw: e.activation(out=o, in_=i, func=func, **kw), r, w)

    def tt(self, eng, out, in0, in1, op):
        r, w = self._rw([out], [in0, in1])
        self.op(eng, lambda e, o=out.ap, a=in0.ap, b=in1.ap: e.tensor_tensor(out=o, in0=a, in1=b, op=op), r, w)

    def ts(self, eng, out, in0, s1, s2, op0, op1=None):
        r, w = self._rw([out], [in0, s1, s2])
        a1, a2 = self._a(s1), self._a(s2)
        if op1 is None:
            self.op(eng, lambda e, o=out.ap, a=in0.ap: e.tensor_scalar(out=o, in0=a, scalar1=a1, scalar2=None, op0=op0), r, w)
        else:
            self.op(eng, lambda e, o=out.ap, a=in0.ap: e.tensor_scalar(out=o, in0=a, scalar1=a1, scalar2=a2, op0=op0, op1=op1), r, w)

    def stt(self, out, in0, scalar, in1, op0, op1):
        r, w = self._rw([out], [in0, scalar, in1])
        s = self._a(scalar)
        self.op("dve", lambda e, o=out.ap, a=in0.ap, b=in1.ap: e.scalar_tensor_tensor(out=o, in0=a, scalar=s, in1=b, op0=op0, op1=op1), r, w)

    def copy(self, eng, out, in_):
        r, w = self._rw([out], [in_])
        if eng == "act":
            self.op("act", lambda e, o=out.ap, i=in_.ap: e.copy(out=o, in_=i), r, w)
        else:
            self.op(eng, lambda e, o=out.ap, i=in_.ap: e.tensor_copy(out=o, in_=i), r, w)

    def memset(self, eng, out, val):
        r, w = self._rw([out], [])
        self.op(eng, lambda e, o=out.ap: e.memset(o, val), r, w)

    def reduce(self, out, in_, op, axis=AX.X):
        r, w = self._rw([out], [in_])
        self.op("dve", lambda e, o=out.ap, i=in_.ap: e.tensor_reduce(out=o, in_=i, axis=axis, op=op), r, w)

    def recip(self, out, in_):
        r, w = self._rw([out], [in_])
        self.op("dve", lambda e, o=out.ap, i=in_.ap: e.reciprocal(out=o, in_=i), r, w)

    def scan(self, out, d0, d1, init, op0, op1):
        r, w = self._rw([out], [d0, d1])
        self.op("dve", lambda e, o=out.ap, a=d0.ap, b=d1.ap: e.tensor_tensor_scan(out=o, data0=a, data1=b, initial=init, op0=op0, op1=op1), r, w)


IN_OFF = dict(hq=0, hf=512, hi=1024, hgate=1536, cq=2048, ckv=2432, kr=2688, ga=2720, gb=3744)


def _blk(w):
    K, n = w.shape
    kc = K // 128
    return np.ascontiguousarray(w.reshape(kc, 128, n).transpose(1, 0, 2)).reshape(128, kc * n)


def _pad(a, width=1024, rows=128):
    out = np.zeros((rows, width), np.float32)
    out[: a.shape[0], : a.shape[1]] = a
    return out


S1_BLOCKS = ([("hq", h) for h in range(4)] + [("hgate", h) for h in range(4)] + [("gb", j) for j in range(8)]
             + [("hf", h) for h in range(4)] + [("hi", h) for h in range(4)]
             + [("cq", j) for j in range(3)] + [("ckv", j) for j in range(2)] + [("kr", 0), ("krs", 0)]
             + [x for h in range(8) for x in (("wq", h), ("wqs", h), ("wk", h))] + [("wv", 0)]
             + [x for j in range(8) for x in (("ga", j), ("bra", j))])
S1_IDX = {b: i for i, b in enumerate(S1_BLOCKS)}


def prep_weights(inp):
    f = lambda a: np.asarray(a, dtype=np.float32)
    w_in, w_q_up, w_kv_up = f(inp["w_in"]), f(inp["w_q_up"]), f(inp["w_kv_up"])
    w_br_a, w_br_b, w_out = f(inp["w_br_a"]), f(inp["w_br_b"]), f(inp["w_out"])
    w_gate, w_up, w_down = f(inp["w_gate"]), f(inp["w_up"]), f(inp["w_down"])
    ada_w = f(inp["ada_w"])
    sw = np.concatenate([np.arange(16, 32), np.arange(0, 16)])
    w1 = np.zeros((DEPTH, len(S1_BLOCKS), 128, 1024), np.float32)
    w3b = np.zeros((DEPTH, 128, 4 * 1024), np.float32)
    w3a = np.zeros((DEPTH, 128, 4 * 1024), np.float32)
    w3o = np.zeros((DEPTH, 8, 128, 1024), np.float32)
    w4 = np.zeros((DEPTH, NEXP, 12, 128, 1024), np.float32)
    adaw = np.zeros((DEPTH, 48, 128, 1024), np.float32)
    for l in range(DEPTH):
        for bi, (kind, j) in enumerate(S1_BLOCKS):
            if kind in ("hq", "hf", "hi", "hgate", "cq", "ckv", "ga", "gb"):
                c0 = IN_OFF[kind] + 128 * j
                w1[l, bi] = _blk(w_in[l][:, c0:c0 + 128])
            elif kind in ("kr", "krs"):
                kr = w_in[l][:, 2688:2720]
                if kind == "krs":
                    kr = kr[:, sw]
                m = np.zeros((1024, 128), np.float32)
                m[:, 64:96] = kr
                w1[l, bi] = _blk(m)
            elif kind in ("wq", "wqs"):
                q = w_q_up[l][:, j * 96:(j + 1) * 96]
                m = np.zeros((384, 128), np.float32)
                if kind == "wq":
                    m[:, 0:96] = q
                else:
                    m[:, 64:96] = q[:, 64:96][:, sw]
                w1[l, bi] = _pad(_blk(m))
            elif kind == "wk":
                m = np.zeros((256, 128), np.float32)
                m[:, 0:64] = w_kv_up[l][:, j * 128:j * 128 + 64]
                w1[l, bi] = _pad(_blk(m))
            elif kind == "wv":
                m = np.zeros((256, 512), np.float32)
                for h in range(8):
                    m[:, h * 64:(h + 1) * 64] = w_kv_up[l][:, h * 128 + 64:h * 128 + 128]
                w1[l, bi] = _blk(m)
            elif kind == "bra":
                w1[l, bi] = _pad(_blk(w_br_a[l][:, j * 128:(j + 1) * 128]))
        w3b[l] = _blk(w_br_b[l])
        w3a[l] = _blk(w_br_a[l])
        for j in range(8):
            w3o[l, j] = w_out[l][j * 128:(j + 1) * 128, :]
        for e in range(NEXP):
            for j in range(4):
                w4[l, e, 2 * j] = _blk(w_gate[l, e][:, j * 128:(j + 1) * 128])
                w4[l, e, 2 * j + 1] = _blk(w_up[l, e][:, j * 128:(j + 1) * 128])
                w4[l, e, 8 + j] = w_down[l, e][j * 128:(j + 1) * 128, :]
        for cg in range(12):
            for q in range(4):
                adaw[l, cg * 4 + q] = _blk(ada_w[l][q * 256:(q + 1) * 256, cg * 512:(cg + 1) * 512])
    w1h = [w1, w1.copy()]
    for half in range(2):
        for l in range(DEPTH):
            for j in range(4):
                gh = half * 4 + j
                q = w_q_up[l][:, gh * 96:(gh + 1) * 96]
                m = np.zeros((384, 128), np.float32)
                m[:, 0:96] = q
                w1h[half][l, S1_IDX[("wq", j)]] = _pad(_blk(m))
                m = np.zeros((384, 128), np.float32)
                m[:, 64:96] = q[:, 64:96][:, sw]
                w1h[half][l, S1_IDX[("wqs", j)]] = _pad(_blk(m))
                m = np.zeros((256, 128), np.float32)
                m[:, 0:64] = w_kv_up[l][:, gh * 128:gh * 128 + 64]
                w1h[half][l, S1_IDX[("wk", j)]] = _pad(_blk(m))
            m = np.zeros((256, 512), np.float32)
            for j in range(4):
                gh = half * 4 + j
                m[:, j * 64:(j + 1) * 64] = w_kv_up[l][:, gh * 128 + 64:gh * 128 + 128]
            w1h[half][l, S1_IDX[("wv", 0)]] = _blk(m)
            for j in range(2):
                gh = half * 2 + j
                for kind in ("hq", "hf", "hi", "hgate"):
                    c0 = IN_OFF[kind] + 128 * gh
                    w1h[half][l, S1_IDX[(kind, j)]] = _blk(w_in[l][:, c0:c0 + 128])
    pv = lambda a, n: np.ascontiguousarray(f(a).reshape(DEPTH, n, 128).transpose(2, 0, 1))
    cstf = np.zeros((128, 1024), np.float32)
    cstf[:, 0:128] = np.eye(128, dtype=np.float32)
    cstf[0:32, 128:160] = np.triu(np.ones((32, 32), np.float32))
    m = np.ones(512, np.float32)
    m[0::32] = 0.0
    cstf[:, 160:672] = m[None, :]
    inv = (np.float32(ROPE_BASE) ** (-np.arange(16, dtype=np.float32) / np.float32(16))).astype(np.float32)
    cstf[64:96, 672] = np.concatenate([inv, inv])
    cstf[64:80, 673] = -1.0
    cstf[80:96, 673] = 1.0
    cstr = np.zeros((128, 256), np.float32)
    cstr[:, 128:256] = 1.0
    return dict(
        w1=w1h, w3b=w3b, w3a=w3a, w3o=w3o, w4=w4, adaw=adaw, cstf=cstf, cstr=cstr,
        adab=f(inp["ada_b"]).reshape(DEPTH, 1, 6 * D),
        n1g=f(inp["norm1_g"]).reshape(DEPTH, 1, D), n2g=f(inp["norm2_g"]).reshape(DEPTH, 1, D),
        fing=f(inp["final_g"]).reshape(1, D),
        lbl=[np.ascontiguousarray(pv(inp["hg_lb_logits"], 4)[:, :, [2 * hf_, 2 * hf_ + 1, 2 * hf_, 2 * hf_ + 1]]) for hf_ in range(2)],
        hgn=[np.ascontiguousarray(pv(inp["hg_norm_g"], 4)[:, :, [2 * hf_, 2 * hf_ + 1, 2 * hf_, 2 * hf_ + 1]]) for hf_ in range(2)],
        qng=pv(inp["q_norm_g"], 3), kvng=pv(inp["kv_norm_g"], 2),
        wrt=_blk(f(inp["w_router"])), rbias=f(inp["router_bias"]).reshape(1, NEXP),
    )


def build(T, nlayers=DEPTH, upto="all", dbg=False):
    nc = bass.Bass("TRN2", target_bir_lowering=False)
    nc.dge_precook = False
    NT = T // 512
    NKC = T // 128
    P = Prog(nc)

    def din(name, shape, dt=F32):
        return V(nc.dram_tensor(name, list(shape), dt, kind="ExternalInput").ap(), name)

    def dscr(name, shape, dt=F32, out=False):
        kind = "ExternalOutput" if (out or dbg) else "Internal"
        return V(nc.dram_tensor(name, list(shape), dt, kind=kind).ap(), name)

    x_in = din("x", [T, D])
    cvec = din("cvec", [128, 8])
    pos = din("pos", [1, T], I32)
    w1 = din("w1", [DEPTH, len(S1_BLOCKS), 128, 1024], F32R)
    w3b = din("w3b", [DEPTH, 128, 4 * 1024], F32R)
    w3a = din("w3a", [DEPTH, 128, 4 * 1024], F32R)
    w3o = din("w3o", [DEPTH, 8, 128, 1024], F32R)
    w4 = din("w4", [DEPTH, NEXP, 12, 128, 1024], F32R)
    adaw = din("adaw", [DEPTH, 48, 128, 1024], F32R)
    cstf_d = din("cstf", [128, 1024])
    cstr_d = din("cstr", [128, 256], F32R)
    adab = din("adab", [DEPTH, 1, 6 * D])
    n1g = din("n1g", [DEPTH, 1, D])
    n2g = din("n2g", [DEPTH, 1, D])
    fing = din("fing", [1, D])
    lbl_d = din("lbl", [128, DEPTH, 4])
    hgn_d = din("hgn", [128, DEPTH, 4])
    qng_d = din("qng", [128, DEPTH, 3])
    kvng_d = din("kvng", [128, DEPTH, 2])
    wrt_d = din("wrt", [128, 8 * NEXP], F32R)
    rbias_d = din("rbias", [1, NEXP])
    hsel_d = din("hsel", [128, 2], I32)

    TH = T // 2
    out_d = dscr("out", [TH, D], out=True)
    xa = dscr("xa", [T, D])
    CR = 512
    NCH = TH // CR
    xg = [dscr("xg%d" % c, [2 * CR, D]) for c in range(NCH)]
    xh_c = [dscr("xh%d" % c, [CR, D]) for c in range(NCH)]

    def xblk(l, bi):
        if l == 0:
            return x_in[bi * 128:(bi + 1) * 128, :].sub(("x", bi))
        h, wr = divmod(bi * 128, TH)
        c, r = divmod(wr, CR)
        return xg[c][h * CR + r:h * CR + r + 128, :].sub(("xb", bi))
    modd = dscr("modd", [DEPTH, 1, 6 * D])
    rot = dscr("rot", [2, 32, T])
    ma_d = dscr("ma", [D, T])
    gb_d = dscr("gb", [D, T])
    qT_d = dscr("qT", [MLA_H, 96, T], F32R)
    kT_d = dscr("kT", [MLA_H, 96, T], F32R)
    vA_d = dscr("vA", [MLA_H, 128, NKC, 65], F32R)
    NHL = MLA_H // 2
    NGL = HG_H // 2
    onm = [dscr("onm%d" % c, [128, T]) for c in range(NGL)]
    ong = [dscr("ong%d" % c, [256, T]) for c in range(NGL)]
    atm = [dscr("atm%d" % c, [128, T]) for c in range(2)]
    atg = [dscr("atg%d" % c, [256, T]) for c in range(2)]

    cstf = P.sb("cstf", [128, 1024])
    cstr = P.sb("cstr", [128, 256], F32R)
    ident = cstf[:, 0:128]
    tri = cstf[0:32, 128:160]
    scanmask = cstf[:, 160:672]
    inv_c = cstf[:, 672:673]
    sgn_c = cstf[:, 673:674]
    onesR = cstr[:, 128:256]
    lb = P.sb("lb", [128, DEPTH, 4])
    oml = P.sb("oml", [128, DEPTH, 4])
    hgn = P.sb("hgn", [128, DEPTH, 4])
    qng = P.sb("qng", [128, DEPTH, 3])
    kvng = P.sb("kvng", [128, DEPTH, 2])
    A_t = P.sb("A_t", [128, D])
    B_t = P.sb("B_t", [128, D])
    G_t = P.sb("G_t", [128, D])
    ring = []
    rcnt = [0]
    pb = [P.ps("pb%d" % i, [128, 512]) for i in range(8)]

    live = [1]

    def make_ring(n, nlive=1):
        live[0] = nlive
        del ring[:]
        ring.extend(P.sb("ring%d" % i, [128, 1024], F32R) for i in range(n))
        rcnt[0] = 0

    wplan = {"seq": [], "issued": 0, "used": 0}

    def plan_weights(fn):
        P.dry = True
        wplan["seq"], wplan["issued"], wplan["used"] = [], 0, 0
        fn()
        P.dry = False

    def pool_dma(fn_make, rd, wr, coll=False):
        if P.dry:
            return
        if coll:
            k, dinc = P.ndma, 1
        else:
            k, dinc = P.dn % P.ndma, 16
            P.dn += 1
        deps = P._deps(rd, wr)
        if P.dcnt[k] > 0:
            deps.add(("d", k, P.dcnt[k]))
        waits = P._resolve("pool", deps, True)
        P.dcnt[k] += dinc
        val = P.dcnt[k]
        P.stream["pool"].append({"fn": fn_make, "waits": waits, "inc": False, "dma": k, "dinc": dinc,
                                 "snap": dict(P.know["pool"])})
        P.dsnap[(k, val)] = dict(P.know["pool"])
        P._register(("d", k, val), rd, wr)

    def gather_rows(out, in_, idx, bound, reads=()):
        io = bass.IndirectOffsetOnAxis(ap=idx.ap, axis=0)
        pool_dma(lambda eng, o=out.ap, i=in_.ap: eng.indirect_dma_start(out=o, out_offset=None, in_=i, in_offset=io,
                                                                      bounds_check=bound, oob_is_err=False),
                 [idx.tok] + list(reads), [out.tok])

    def getw(src, width, parts=128):
        if P.dry:
            wplan["seq"].append((src, width, parts))
            return ring[0]
        i = wplan["used"]
        assert wplan["seq"][i][1] == width and wplan["seq"][i][2] == parts
        NR = len(ring)
        while wplan["issued"] < min(len(wplan["seq"]), i + NR - (live[0] - 1)):
            j = wplan["issued"]
            s_, w_, p_ = wplan["seq"][j]
            slot = ring[(rcnt[0] + j) % NR]
            P.dma("sp", slot[0:p_, 0:w_], s_[0:p_, 0:w_])
            wplan["issued"] += 1
        slot = ring[(rcnt[0] + i) % NR]
        wplan["used"] += 1
        if wplan["used"] == len(wplan["seq"]):
            rcnt[0] += len(wplan["seq"])
            wplan["seq"], wplan["issued"], wplan["used"] = [], 0, 0
        return slot

    P.dma("sp", cstf, cstf_d)
    P.dma("sp", cstr, cstr_d)
    P.dma("sp", hgn, hgn_d)
    P.dma("sp", qng, qng_d)
    P.dma("sp", kvng, kvng_d)

    eps_t = P.sb("eps_t", [128, 1])
    P.memset("dve", eps_t, EPS)

    P.push()
    lbl = P.sb("lbl", [128, DEPTH, 4])
    P.dma("sp", lbl, lbl_d)
    t4 = P.sb("t4", [128, 4])
    P.tt("dve", t4, lbl[:, 1, :], lbl[:, 0, :], ALU.subtract)
    P.memset("dve", lb[:, 0, :], 0.0)
    P.act(lb[:, 1, :], t4, AF.Sigmoid)
    P.ts("dve", oml, lb, -1.0, 1.0, ALU.mult, ALU.add)

    pi_ = P.sb("pi", [128, 512], I32)
    pf = P.sb("pf", [128, 512])
    ang = P.sb("ang", [128, 512])
    rr = [P.sb("rr", [128, 512]) for _ in range(2)]
    tb = [P.sb("tb", [128, 512]) for _ in range(2)]
    for n in range(NT):
        P.dma("sp", pi_[64:96, :], pos[:, n * 512:(n + 1) * 512].bto([32, 512]))
        P.copy("dve", pf[64:96, :], pi_[64:96, :])
        P.ts("dve", ang[64:96, :], pf[64:96, :], inv_c[64:96, :], None, ALU.mult)
        for which, shift in ((0, np.pi / 2), (1, 0.0)):
            r_, t_ = rr[which], tb[which]
            a2, kk_, mm_ = tb[which], pf, pi_.bc(F32)
            P.ts("dve", a2[64:96, :], ang[64:96, :], float(shift), None, ALU.add)
            P.ts("dve", kk_[64:96, :], a2[64:96, :], float(1.0 / TWO_PI), 12582912.0, ALU.mult, ALU.add)
            P.ts("dve", kk_[64:96, :], kk_[64:96, :], -12582912.0, None, ALU.add)
            P.stt(r_[64:96, :], kk_[64:96, :], -6.28125, a2[64:96, :], ALU.mult, ALU.add)
            P.stt(r_[64:96, :], kk_[64:96, :], float(-(TWO_PI - 6.28125)), r_[64:96, :], ALU.mult, ALU.add)
            P.ts("dve", mm_[64:96, :], r_[64:96, :], float(np.pi), float(-TWO_PI), ALU.is_gt, ALU.mult)
            P.tt("dve", r_[64:96, :], r_[64:96, :], mm_[64:96, :], ALU.add)
            P.ts("dve", mm_[64:96, :], r_[64:96, :], float(-np.pi), float(TWO_PI), ALU.is_lt, ALU.mult)
            P.tt("dve", r_[64:96, :], r_[64:96, :], mm_[64:96, :], ALU.add)
            P.ts("dve", r_[64:96, :], r_[64:96, :], float(-np.pi), float(np.pi), ALU.max, ALU.min)
            P.act(t_[64:96, :], r_[64:96, :], AF.Sin)
            if which == 1:
                P.ts("dve", t_[64:96, :], t_[64:96, :], sgn_c[64:96, :], None, ALU.mult)
            P.dma("sp", rot[which, :, n * 512:(n + 1) * 512].sub(("rot", n)), t_[64:96, :])
    P.pop()

    P.push()
    cv = P.sb("cv", [128, 8])
    cact = P.sb("cact", [128, 8], F32R)
    make_ring(6)
    P.dma("sp", cv, cvec)
    sg = P.sb("sg", [128, 8])
    P.act(sg, cv, AF.Sigmoid)
    P.tt("dve", cact, cv, sg, ALU.mult)
    ab = P.sb("ab", [1, 6 * D])
    mrow = P.sb("mrow", [1, 6 * D])

    def mod_layer(l):
        P.dma("sp", ab, adab[l])
        for cg in range(12):
            acc = pb[cg % 2]
            for q in range(4):
                slot = getw(adaw[l, cg * 4 + q], 1024)
                for k2 in range(2):
                    kc = q * 2 + k2
                    P.mm(acc[0:1, :], cact[:, kc:kc + 1], slot[:, k2 * 512:(k2 + 1) * 512],
                         start=(kc == 0), stop=(kc == 7))
            P.tt("dve", mrow[:, cg * 512:(cg + 1) * 512], acc[0:1, :], ab[:, cg * 512:(cg + 1) * 512], ALU.add)
        P.dma("sp", modd[l].sub(("modd", l)), mrow)

    for l in range(nlayers):
        plan_weights(lambda: mod_layer(l))
        mod_layer(l)
    P.pop()

    def load_mod(l, which):
        ng = n1g if which == 0 else n2g
        o = 3 * D * which
        md = modd[l].sub(("modd", l))
        P.dma("sp", B_t, md[:, o:o + D].bto([128, D]))
        P.dma("sp", A_t, md[:, o + D:o + 2 * D].bto([128, D]))
        P.dma("sp", G_t, md[:, o + 2 * D:o + 3 * D].bto([128, D]))
        P.push()
        gt = P.sb("gt", [128, D])
        P.dma("sp", gt, ng[l].bto([128, D]))
        P.stt(A_t, A_t, 1.0, gt, ALU.add, ALU.mult)
        P.pop()

    def norm_mod(xt, h_out, junk, ss):
        P.act(junk, xt, AF.Square, accum=ss)
        P.act(ss, ss, AF.Ln, scale=1.0 / D, bias=eps_t)
        P.act(ss, ss, AF.Exp, scale=-0.5)
        P.stt(h_out, xt, ss, A_t, ALU.mult, ALU.mult)
        P.tt("dve", h_out, h_out, B_t, ALU.add)

    R = lambda v: v.bc(F32R)
    nbc = [0]

    def nb():
        b = pb[nbc[0] % 3]
        nbc[0] += 1
        return b

    QSCALE = 0.5 * 128 ** -0.5

    def stage1(l):
        load_mod(l, 0)
        P.push()
        make_ring(6)
        xt = [P.sb("xt", [128, D]) for _ in range(2)]
        hh = [P.sb("hh", [128, D]) for _ in range(2)]
        junk = P.sb("junk", [128, D])
        ss = P.sb("ss", [128, 1])
        hT = P.sb("hT", [128, 8, 512])
        tl = lambda nm: P.sb(nm, [128, 512])
        qs = [tl("qs") for _ in range(4)]
        sgate = [tl("sgate") for _ in range(4)]
        tt_ = tl("tt_")
        gbt = [tl("gbt") for _ in range(2)]
        hgA = P.sb("hgA", [128, 9, 512])
        hgR = P.sb("hgR", [128, 8, 512])
        aslot = lambda i: hgA[:, i, :].sub(("hgA", i))
        rslot = lambda i: hgR[:, i, :].sub(("hgR", i))
        t1, t2, lf, kk, bb, eb, enb, kd, vt = [aslot(i) for i in range(9)]
        kh, qh = rslot(0), rslot(1)
        o_t, sq, t_l, t_on, tmpA, tmpB, kpe, Ct, St = [tl("m%d" % i) for i in range(9)]
        kdT = P.sb("kdT", [32, 16, 128])
        vT = P.sb("vT", [32, 16, 128])
        aT = P.sb("aT", [32, 16, 32])
        Swork = P.sb("Swork", [128, 17, 128])
        Scar = P.sb("Scar", [128, 4, 128])
        ebl = P.sb("ebl", [128, 16])
        onT = P.sb("onT", [128, 4, 512])
        cq = [aslot(i) for i in range(3)]
        sqq = [rslot(2 + i) for i in range(3)]
        cqn = [rslot(5 + i) for i in range(3)]
        ckvn = [rslot(4), rslot(3)]
        qtb = [tl("qt") for _ in range(2)]
        ktb = [tl("kt") for _ in range(2)]
        va = P.sb("va", [128, 8, 4, 65])
        gat = gbt
        mat = [o_t, t_on]
        tri3 = tri.re("p (o t) -> p o t", o=1).bcast([32, 16, 32])
        P.memset("dve", Scar, 0.0)
        P.memset("dve", va, 1.0)
        Sw = lambda c: Swork[:, c, :].sub(("Sw", c))

        def proj(kind, j):
            bank = nb()
            slot = getw(w1[l, S1_IDX[(kind, j)]], 1024)
            for kc in range(8):
                P.mm(bank, slot[:, kc * 128:(kc + 1) * 128], R(hT[:, kc, :]), start=(kc == 0), stop=(kc == 7))
            return bank

        def core(h, mid=None):
            for src_t, dst in ((kd, kdT), (vt, vT)):
                for g in range(4):
                    bank = nb()
                    for c4 in range(4):
                        c = g * 4 + c4
                        P.tr(bank[0:32, c4 * 128:(c4 + 1) * 128], src_t[:, c * 32:(c + 1) * 32], ident)
                    P.copy("act" if g % 2 else "dve", R(dst[:, g * 4:(g + 1) * 4, :]),
                           bank[0:32, :].re("p (c d) -> p c d", d=128))
            for c in range(16):
                P.mm(pb[3][0:32, c * 32:(c + 1) * 32], R(kh[:, c * 32:(c + 1) * 32]), R(qh[:, c * 32:(c + 1) * 32]))
            P.tt("dve", R(aT), pb[3][0:32, :].re("p (c t) -> p c t", t=32), tri3, ALU.mult)
            P.copy("dve", R(Sw(0)), Scar[:, h, :])
            for g in range(4):
                ubk = pb[4 + g % 2]
                for c4 in range(4):
                    c = g * 4 + c4
                    P.mm(ubk[:, c4 * 128:(c4 + 1) * 128], R(kdT[:, c, :]), R(vT[:, c, :]))
                for c4 in range(4):
                    c = g * 4 + c4
                    P.stt(R(Sw(c + 1)), Sw(c), ebl[:, c:c + 1], ubk[:, c4 * 128:(c4 + 1) * 128], ALU.mult, ALU.add)
            P.copy("dve", Scar[:, h, :], Sw(16))
            if mid is not None:
                mid()
            for c in range(16):
                ob = pb[6][:, c * 32:(c + 1) * 32]
                P.mm(ob, R(vT[:, c, :]), R(aT[:, c, :]), start=True, stop=False)
                P.mm(ob, R(Sw(c)), R(qh[:, c * 32:(c + 1) * 32]), start=False, stop=True)
            P.copy("act", o_t, pb[6])
            P.act(R(sq), pb[6], AF.Square)
            P.mm(pb[7], onesR, R(sq))
            P.act(t_l, pb[7], AF.Ln, scale=1.0 / 128, bias=eps_t)
            P.act(t_l, t_l, AF.Exp, scale=-0.5)
            P.stt(t_on, o_t, hgn[:, l, h:h + 1], t_l, ALU.mult, ALU.mult)
            P.stt(R(onT[:, h, :]), sgate[h], 0.5, t_on, ALU.mult, ALU.mult)
            P.dma("sp", onm[h][:, cur["cols"]].sub(("on", h, cur["n"])), onT[:, h, :])

        cur = {}

        def tile(n):
            cols = slice(n * 512, (n + 1) * 512)
            cur["cols"], cur["n"] = cols, n
            P.dma("sp", Ct[64:96, :], rot[0, :, cols].sub(("rot", n)))
            P.dma("sp", St[64:96, :], rot[1, :, cols].sub(("rot", n)))
            for s in range(4):
                r0 = (n * 4 + s) * 128
                P.dma("sp", xt[s % 2], xblk(l, n * 4 + s))
                norm_mod(xt[s % 2], hh[s % 2], junk, ss)
                for half in range(2):
                    bank = nb()
                    for k4 in range(4):
                        kc = half * 4 + k4
                        P.tr(bank[:, k4 * 128:(k4 + 1) * 128], hh[s % 2][:, kc * 128:(kc + 1) * 128], ident)
                    P.copy("act" if half else "dve", R(hT[:, half * 4:(half + 1) * 4, s * 128:(s + 1) * 128]),
                           bank.re("p (k t) -> p k t", t=128))
            def mla_latents():
                for name, nj, gv, dst in (("cq", 3, qng, cqn), ("ckv", 2, kvng, ckvn)):
                    for j in range(nj):
                        bank = proj(name, j)
                        P.copy("act", cq[j], bank)
                        P.act(R(sqq[j]), bank, AF.Square)
                    for j in range(nj):
                        P.mm(pb[7], onesR, R(sqq[j]), start=(j == 0), stop=(j == nj - 1))
                    P.act(t_l, pb[7], AF.Ln, scale=1.0 / (128 * nj), bias=eps_t)
                    P.act(t_l, t_l, AF.Exp, scale=-0.5)
                    for j in range(nj):
                        P.stt(R(dst[j]), cq[j], gv[:, l, j:j + 1], t_l, ALU.mult, ALU.mult)
                b1 = proj("kr", 0)
                b2 = proj("krs", 0)
                P.tt("dve", kpe[64:96, :], b1[64:96, :], Ct[64:96, :], ALU.mult)
                P.tt("dve", tmpA[64:96, :], b2[64:96, :], St[64:96, :], ALU.mult)
                P.tt("dve", kpe[64:96, :], kpe[64:96, :], tmpA[64:96, :], ALU.add)

            def mla_head(h):
                qt, kt = qtb[h % 2], ktb[h % 2]
                bq, bq2 = nb(), nb()
                for kind, bank in (("wq", bq), ("wqs", bq2)):
                    slot = getw(w1[l, S1_IDX[(kind, h)]], 384)
                    for kc in range(3):
                        P.mm(bank, slot[:, kc * 128:(kc + 1) * 128], R(cqn[kc]), start=(kc == 0), stop=(kc == 2))
                P.copy("act", R(qt[0:64, :]), bq[0:64, :])
                P.tt("dve", tmpA[64:96, :], bq[64:96, :], Ct[64:96, :], ALU.mult)
                P.tt("dve", tmpB[64:96, :], bq2[64:96, :], St[64:96, :], ALU.mult)
                P.tt("dve", R(qt[64:96, :]), tmpA[64:96, :], tmpB[64:96, :], ALU.add)
                P.dma("sp", qT_d[h, :, cols].bc(F32).sub(("qT", h, n)), qt[0:96, :])
                bk = nb()
                slot = getw(w1[l, S1_IDX[("wk", h)]], 256)
                for kc in range(2):
                    P.mm(bk, slot[:, kc * 128:(kc + 1) * 128], R(ckvn[kc]), start=(kc == 0), stop=(kc == 1))
                P.copy("act", R(kt[0:64, :]), bk[0:64, :])
                P.copy("pool", R(kt[64:96, :]), kpe[64:96, :])
                P.dma("sp", kT_d[h, :, cols].bc(F32).sub(("kT", h)), kt[0:96, :])

            def mla_v():
                slot = getw(w1[l, S1_IDX[("wv", 0)]], 1024)
                for s in range(4):
                    bv = nb()
                    for kc in range(2):
                        P.mm(bv[:, 0:NHL * 64], R(ckvn[kc][:, s * 128:(s + 1) * 128]), slot[:, kc * 512:kc * 512 + NHL * 64],
                             start=(kc == 0), stop=(kc == 1))
                    P.copy("act", R(va[:, 0:NHL, s, 0:64]), bv[:, 0:NHL * 64].re("p (h v) -> p h v", v=64))
                for h in range(NHL):
                    P.dma("sp", vA_d[h, :, n * 4:(n + 1) * 4, :].bc(F32).sub(("vA", h)), va[:, h, :, :])

            mla_latents()
            for h in range(NGL):
                bank = proj("hq", h)
                P.act(tt_, bank, AF.Tanh, scale=0.5)
                P.stt(qs[h], tt_, 1.0, bank, ALU.add, ALU.mult)
            for h in range(NGL):
                bank = proj("hgate", h)
                P.act(tt_, bank, AF.Tanh, scale=0.5)
                P.stt(sgate[h], tt_, 1.0, bank, ALU.add, ALU.mult)
            for j in range(8):
                bank = proj("gb", j)
                P.act(tt_, bank, AF.Tanh, scale=0.5)
                P.ts("dve", gbt[j % 2], tt_, 0.5, 0.5, ALU.mult, ALU.add)
                P.dma("sp", gb_d[j * 128:(j + 1) * 128, cols].sub(("gb", j, n)), gbt[j % 2])
            for h in range(NGL):
                bank = proj("hf", h)
                bank_hi = proj("hi", h)
                P.act(t1, bank, AF.Exp, scale=-1.0)
                P.act(t1, t1, AF.Ln, bias=1.0)
                P.act(t2, t1, AF.Exp, scale=-1.0)
                P.ts("dve", t2, t2, oml[:, l, h:h + 1], lb[:, l, h:h + 1], ALU.mult, ALU.add)
                P.act(lf, t2, AF.Ln)
                P.ts("dve", kk, t2, -1.0, 1.0, ALU.mult, ALU.add)
                P.scan(bb, scanmask, lf, 0.0, ALU.mult, ALU.add)
                P.act(eb, bb, AF.Exp)
                P.act(enb, bb, AF.Exp, scale=-1.0)
                P.tt("dve", R(kh), kk, enb, ALU.mult)
                b3 = bb.re("p (c t) -> p c t", t=32)
                P.act(ebl, b3[:, :, 31], AF.Exp)
                P.tt("dve", kd.re("p (c t) -> p c t", t=32), kh.re("p (c t) -> p c t", t=32),
                     ebl.re("p (c o) -> p c o", o=1).bcast([128, 16, 32]), ALU.mult)
                P.stt(R(qh), qs[h], QSCALE, eb, ALU.mult, ALU.mult)
                P.copy("act", vt, bank_hi)
                mla_head(2 * h)
                core(h, mid=lambda h=h: mla_head(2 * h + 1))
            mla_v()
            for j in range(8):
                bank = proj("ga", j)
                P.act(tt_, bank, AF.Tanh, scale=0.5)
                P.ts("dve", gat[j % 2], tt_, 0.5, 0.5, ALU.mult, ALU.add)
                P.dma("sp", ma_d[j * 128:(j + 1) * 128, cols].sub(("ma", j, n)), gat[j % 2])

        def all_tiles():
            for n in range(NT):
                tile(n)

        plan_weights(all_tiles)
        all_tiles()
        for c in range(NGL):
            pool_dma(lambda eng, c=c: eng.collective_compute(
                "AllGather", ALU.bypass, replica_groups=[[0, 1], [2, 3], [4, 5], [6, 7]],
                ins=[onm[c].ap], outs=[ong[c].ap]), [("on", c, n2) for n2 in range(NT)], [("ong", c)], coll=True)
        P.pop()

    def stage2(l):
        P.push()
        SC = float(96 ** -0.5)
        ktl = [P.sb("ktl", [96, T], F32R) for _ in range(2)]
        val = [P.sb("val", [128, NKC, 65], F32R) for _ in range(2)]
        qtl = [P.sb("qtl", [96, 512], F32R) for _ in range(2)]
        pTs = [P.sb("pT", [128, 512]) for _ in range(4)]
        osb = [P.sb("osb", [64, 512]) for _ in range(2)]
        rdn = [P.sb("rdn", [128, 512]) for _ in range(2)]
        rdr = [P.sb("rdr", [128, 512]) for _ in range(2)]
        zt = P.sb("zt", [128, 512])
        P.memset("pool", zt, 0.0)
        ato = [P.sb("ato", [64, 512]) for _ in range(2)]
        cnt = 0
        pending = []

        def norm_tail(h, n, po):
            o_, r_, a_ = osb[n % 2], rdn[n % 2], ato[n % 2]
            P.copy("dve", o_, po[0:64, :])
            P.recip(r_[64:65, :], po[64:65, :])
            r2_ = rdr[n % 2]
            P.copy("dve", R(r2_[64:65, :]), r_[64:65, :])
            pbx = pb[6 + n % 2]
            P.mm(pbx[0:64, :], onesR[64:65, 0:64], R(r2_[64:65, :]))
            P.tt("dve", R(a_), o_, pbx[0:64, :], ALU.mult)
            c = h // 2
            P.dma("sp", atm[c][(h % 2) * 64:(h % 2 + 1) * 64, n * 512:(n + 1) * 512].sub(("at", c, n)), a_)
            if h % 2 == 1 and n == NT - 1:
                pool_dma(lambda eng, c=c: eng.collective_compute(
                    "AllGather", ALU.bypass, replica_groups=[[0, 1], [2, 3], [4, 5], [6, 7]],
                    ins=[atm[c].ap], outs=[atg[c].ap]), [("at", c, n2) for n2 in range(NT)], [("atg", c)], coll=True)

        for h in range(NHL):
            kt_, va_ = ktl[h % 2], val[h % 2]
            P.dma("sp", kt_, kT_d[h].sub(("kT", h)))
            P.dma("sp", va_, vA_d[h].sub(("vA", h)))
            for n in range(NT):
                qt_ = qtl[n % 2]
                P.dma("sp", qt_, qT_d[h, :, n * 512:(n + 1) * 512].sub(("qT", h, n)))
                po = pb[4 + (n % 2)]
                nk = 4 * n + 4
                AHEAD = 2

                def score(j):
                    c0 = max(j - 4 * n, 0) * 128
                    ps = pb[(cnt + j) % 4]
                    P.mm(ps[:, c0:512], kt_[:, j * 128:(j + 1) * 128], qt_[:, c0:512])

                for j in range(min(AHEAD, nk)):
                    score(j)
                for j in range(nk):
                    jj = j - 4 * n
                    c0 = max(jj, 0) * 128
                    ps = pb[(cnt + j) % 4]
                    pt = pTs[(cnt + j) % 4]
                    if j + AHEAD < nk:
                        score(j + AHEAD)
                    P.act(R(pt[:, c0:512]), ps[:, c0:512], AF.Exp, scale=SC)
                    if jj >= 0:
                        if c0 > 0:
                            P.copy("dve", R(pt[:, 0:c0]), zt[:, 0:c0])
                        P.copy("dve", R(pt[64:128, c0:c0 + 64]), zt[64:128, 0:64])
                    P.mm(po[0:65, :], va_[:, j, :], R(pt), start=(j == 0), stop=(j == nk - 1))
                    if j == min(2, nk - 1):
                        while pending:
                            norm_tail(*pending.pop(0))
                cnt += nk
                pending.append((h, n, po))
        while pending:
            norm_tail(*pending.pop(0))
        P.pop()

    def stage3(l, dst):
        P.push()
        wbb = P.sb("wbb", [128, 4, 1024], F32R)
        wo = P.sb("wo", [128, 8, 1024], F32R)
        P.dma("sp", wbb.re("p c n -> p (c n)"), w3b[l])
        for kc in range(8):
            P.dma("sp", wo[:, kc, :], w3o[l, kc])
            P.tt("pool" if kc % 2 else "dve", wo[:, kc, :], wo[:, kc, :], G_t, ALU.mult)
        wba = P.sb("wba", [128, 4, 1024], F32R)
        P.dma("sp", wba.re("p c n -> p (c n)"), w3a[l])
        att = [P.sb("att", [128, 4, 512], F32R) for _ in range(2)]
        ont = [P.sb("ont", [128, 4, 512], F32R) for _ in range(2)]
        mat_ = [P.sb("mat3", [128, 512]) for _ in range(2)]
        gbt_ = [P.sb("gbt3", [128, 512]) for _ in range(2)]
        tmp = [P.sb("tmp3", [128, 512]) for _ in range(2)]
        mg = P.sb("mg", [128, 8, 512])
        xt = [P.sb("xt3", [128, D]) for _ in range(2)]
        xo = [P.sb("xo3", [128, D]) for _ in range(2)]
        for n in range(NT):
            cols = slice(n * 512, (n + 1) * 512)
            a_ = att[n % 2]
            for c in range(4):
                P.dma("sp", a_[:, c, :], atg[c % 2][(c // 2) * 128:(c // 2 + 1) * 128, cols].bc(F32R).sub(("atg", c % 2)))
            o_ = ont[n % 2]
            for gh in range(4):
                P.dma("sp", o_[:, gh, :], ong[gh % 2][(gh // 2) * 128:(gh // 2 + 1) * 128, cols].bc(F32R).sub(("ong", gh % 2)))
            for j in range(8):
                P.dma("sp", mat_[j % 2], ma_d[j * 128:(j + 1) * 128, cols].sub(("ma", j, n)))
                P.dma("sp", gbt_[j % 2], gb_d[j * 128:(j + 1) * 128, cols].sub(("gb", j, n)))
                bka = nb()
                for gh in range(4):
                    P.mm(bka, wba[:, gh, j * 128:(j + 1) * 128], o_[:, gh, :], start=(gh == 0), stop=(gh == 3))
                P.tt("dve", mat_[j % 2], bka, mat_[j % 2], ALU.mult)
                bank = nb()
                for kc in range(4):
                    P.mm(bank, wbb[:, kc, j * 128:(j + 1) * 128], a_[:, kc, :], start=(kc == 0), stop=(kc == 3))
                P.tt("dve", tmp[j % 2], bank, gbt_[j % 2], ALU.mult)
                P.tt("pool", R(mg[:, j, :]), tmp[j % 2], mat_[j % 2], ALU.add)
            for s in range(4):
                r0 = n * 512 + s * 128
                bi = n * 4 + s
                P.dma("sp", xt[s % 2], xblk(l, bi))
                for half in range(2):
                    hs = slice(half * 512, (half + 1) * 512)
                    bank = nb()
                    for j in range(8):
                        P.mm(bank, R(mg[:, j, s * 128:(s + 1) * 128]), wo[:, j, hs], start=(j == 0), stop=(j == 7))
                    P.tt("dve", xo[s % 2][:, hs], bank, xt[s % 2][:, hs], ALU.add)
                P.dma("sp", dst[r0:r0 + 128, :].sub((dst.tok, bi)), xo[s % 2])
        P.pop()

    def stage4(l, src, dst, last):
        load_mod(l, 1)
        P.push()
        BIG = 1.0e4
        MT = 1024
        NB = MT // 128
        make_ring(12, 4)
        wrt = P.sb("wrt", [128, 8 * NEXP], F32R)
        rb = P.sb("rb", [128, NEXP])
        P.dma("sp", wrt, wrt_d)
        P.dma("sp", rb, rbias_d.bto([128, NEXP]))
        if last:
            FG = P.sb("FG", [128, D])
            P.dma("sp", FG, fing.bto([128, D]))
        hsel = P.sb("hsel", [128, 2], I32)
        P.dma("sp", hsel, hsel_d)
        xm = P.sb("xm", [128, NB, D])
        hh = [P.sb("hh4", [128, D]) for _ in range(2)]
        junk = P.sb("junk4", [128, D])
        ss = P.sb("ss4", [128, 1])
        h2T = P.sb("h2T", [128, 8, MT])
        yacc = P.sb("yacc", [128, NB, D])
        heT = P.sb("heT", [128, 4, MT])
        sil = [P.sb("sil", [128, 512]) for _ in range(2)]
        comb = P.sb("comb", [128, NB, NEXP])
        sm = lambda nm, w: P.sb(nm, [128, w])
        sc_, bi_, eq_, b2_, mk_, q1_, mk2_, q2_, ws_ = [sm("r%d" % i, 16) for i in range(9)]
        m1_, m2_, gs_, gsel_, pen_ = [sm("g%d" % i, 4) for i in range(5)]
        gm_, e1_, e2_, den_, rden_ = [sm("s%d" % i, 1) for i in range(5)]
        g3 = lambda v: v.re("p (g e) -> p g e", e=4)

        def route(blk, lg):
            P.act(sc_, lg, AF.Sigmoid)
            P.tt("dve", bi_, sc_, rb, ALU.add)
            P.reduce(m1_, g3(bi_), ALU.max)
            P.tt("dve", g3(eq_), g3(bi_), m1_.re("p (g o) -> p g o", o=1).bcast([128, 4, 4]), ALU.is_equal)
            P.stt(b2_, eq_, -BIG, bi_, ALU.mult, ALU.add)
            P.reduce(m2_, g3(b2_), ALU.max)
            P.tt("dve", gs_, m1_, m2_, ALU.add)
            P.reduce(gm_, gs_, ALU.max)
            P.ts("dve", gsel_, gs_, gm_, None, ALU.is_equal)
            P.ts("dve", pen_, gsel_, 1.0, BIG, ALU.subtract, ALU.mult)
            P.tt("dve", g3(mk_), g3(bi_), pen_.re("p (g o) -> p g o", o=1).bcast([128, 4, 4]), ALU.add)
            P.reduce(e1_, mk_, ALU.max)
            P.ts("dve", q1_, mk_, e1_, None, ALU.is_equal)
            P.stt(mk2_, q1_, -BIG, mk_, ALU.mult, ALU.add)
            P.reduce(e2_, mk2_, ALU.max)
            P.ts("dve", q2_, mk2_, e2_, None, ALU.is_equal)
            P.tt("dve", q1_, q1_, q2_, ALU.add)
            P.tt("dve", ws_, sc_, q1_, ALU.mult)
            P.reduce(den_, ws_, ALU.add)
            P.recip(rden_, den_)
            P.ts("dve", comb[:, blk, :], ws_, rden_, None, ALU.mult)

        def macro(m):
            gather_rows(xm.re("p b n -> p (b n)"), src.re("(s r) n -> s (r n)", r=NB), hsel[:, m:m + 1], T // NB - 1,
                        reads=[(src.tok, bi) for bi in range(T // 128)])
            for b in range(NB):
                norm_mod(xm[:, b, :], hh[b % 2], junk, ss)
                for half in range(2):
                    bank = nb()
                    for k4 in range(4):
                        kc = half * 4 + k4
                        P.tr(bank[:, k4 * 128:(k4 + 1) * 128], hh[b % 2][:, kc * 128:(kc + 1) * 128], ident)
                    P.copy("act" if half else "dve", R(h2T[:, half * 4:(half + 1) * 4, b * 128:(b + 1) * 128]),
                           bank.re("p (k t) -> p k t", t=128))
                for kc in range(8):
                    P.mm(pb[7][:, 0:NEXP], R(h2T[:, kc, b * 128:(b + 1) * 128]), wrt[:, kc * NEXP:(kc + 1) * NEXP],
                         start=(kc == 0), stop=(kc == 7))
                route(b, pb[7][:, 0:NEXP])
            for e in range(NEXP):
                for j in range(4):
                    sg_ = getw(w4[l, e, 2 * j], 1024)
                    su_ = getw(w4[l, e, 2 * j + 1], 1024)
                    for sub in range(MT // 512):
                        ts_ = slice(sub * 512, (sub + 1) * 512)
                        bg, bu = nb(), pb[3 + (j * 2 + sub) % 2]
                        for kc in range(8):
                            P.mm(bg, sg_[:, kc * 128:(kc + 1) * 128], R(h2T[:, kc, ts_]), start=(kc == 0), stop=(kc == 7))
                        for kc in range(8):
                            P.mm(bu, su_[:, kc * 128:(kc + 1) * 128], R(h2T[:, kc, ts_]), start=(kc == 0), stop=(kc == 7))
                        s_ = sil[sub % 2]
                        P.act(s_, bg, AF.Silu)
                        P.tt("dve", R(heT[:, j, ts_]), s_, bu, ALU.mult)
                sd = [getw(w4[l, e, 8 + j], 1024) for j in range(4)]
                for b in range(NB):
                    for half in range(2):
                        hs = slice(half * 512, (half + 1) * 512)
                        bank = pb[5 + (b * 2 + half) % 2]
                        for j in range(4):
                            P.mm(bank, R(heT[:, j, b * 128:(b + 1) * 128]), sd[j][:, hs], start=(j == 0), stop=(j == 3))
                        if e == 0:
                            P.ts("dve", yacc[:, b, hs], bank, comb[:, b, 0:1], None, ALU.mult)
                        else:
                            P.stt(yacc[:, b, hs], bank, comb[:, b, e:e + 1], yacc[:, b, hs], ALU.mult, ALU.add)
            dview = dst.re("(m p r) n -> m p r n", p=128, r=NB) if last else None
            for b in range(NB):
                P.tt("dve", yacc[:, b, :], yacc[:, b, :], G_t, ALU.mult)
                P.tt("dve", hh[b % 2], yacc[:, b, :], xm[:, b, :], ALU.add)
                dtk = (dst.tok, m, b)
                if not last:
                    for hc in range(2):
                        cv = xh_c[2 * m + hc].re("(p r) n -> p r n", r=NB)
                        P.dma("sp", cv[:, b, :].sub(("xh", m, b, hc)), hh[b % 2][hc * 64:(hc + 1) * 64, :])
                else:
                    P.act(junk, hh[b % 2], AF.Square, accum=ss)
                    P.act(ss, ss, AF.Ln, scale=1.0 / D, bias=eps_t)
                    P.act(ss, ss, AF.Exp, scale=-0.5)
                    P.stt(yacc[:, b, :], hh[b % 2], ss, FG, ALU.mult, ALU.mult)
                    P.dma("sp", dview[m][:, b, :].sub(dtk), yacc[:, b, :])

        NM = TH // MT

        def all_macros():
            for m in range(NM):
                macro(m)
                if not last:
                    for c in range(m * MT // CR, (m + 1) * MT // CR):
                        rd = [("xh", m, b, c % 2) for b in range(NB)]
                        wr = [("xb", (h * TH + c * CR) // 128 + q) for h in range(2) for q in range(CR // 128)]
                        pool_dma(lambda eng, c=c: eng.collective_compute(
                            "AllGather", ALU.bypass, replica_groups=[[0, 1], [2, 3], [4, 5], [6, 7]],
                            ins=[xh_c[c].ap], outs=[xg[c].ap]), rd, wr, coll=True)

        plan_weights(all_macros)
        all_macros()
        P.pop()

    for l in range(nlayers):
        stage1(l)
        if upto == "s1":
            break
        stage2(l)
        if upto == "s2":
            break
        stage3(l, xa)
        if upto == "s3":
            break
        last = (l == nlayers - 1)
        stage4(l, xa, out_d, last)
    P.barrier()
    P.emit()
    P.close()
    return nc


def core_inputs(inp, W, b, T, half=0):
    f = lambda a: np.asarray(a, dtype=np.float32)
    d = dict(W)
    d["w1"] = W["w1"][half]
    d["lbl"] = W["lbl"][half]
    d["hgn"] = W["hgn"][half]
    d["x"] = np.ascontiguousarray(f(inp["x"])[b, :T])
    d["cvec"] = np.ascontiguousarray(f(inp["c"])[b].reshape(8, 128).T)
    d["pos"] = np.ascontiguousarray(np.asarray(inp["positions"], dtype=np.int32)[b:b + 1, :T])
    sup = (T // 2) // 8
    d["hsel"] = np.ascontiguousarray((half * sup + np.arange(2)[None, :] * 128 + np.arange(128)[:, None]).astype(np.int32))
    return d


def kernel(**inputs):
    T = 4096
    W = prep_weights(inputs)
    nc = build(T)
    in_maps = [core_inputs(inputs, W, i // 2, T, half=i % 2) for i in range(8)]
    res = run_bass_kernel_spmd(nc, in_maps, core_ids=list(range(8)))
    out = np.stack([np.concatenate([np.asarray(res.results[2 * b + h]["out"], dtype=np.float32) for h in range(2)], axis=0)
                    for b in range(4)], axis=0)
    return out
```

```python
import numpy as np
from contextlib import ExitStack
import concourse.bass as bass
import concourse.mybir as mybir
from concourse.bass_utils import run_bass_kernel_spmd

F32 = mybir.dt.float32
F32R = mybir.dt.float32r
I32 = mybir.dt.int32
ALU = mybir.AluOpType
AF = mybir.ActivationFunctionType
AX = mybir.AxisListType

ENGS = ("pe", "act", "dve", "pool", "sp")

D = 1024
DEPTH = 2
HG_H = 4
MLA_H = 8
NEXP = 16
EPS = 1e-6
ROPE_BASE = 10000.0
TWO_PI = 2.0 * np.pi


class V:
    def __init__(self, ap, tok):
        self.ap = ap
        self.tok = tok

    def __getitem__(self, k):
        return V(self.ap[k], self.tok)

    def bc(self, dt):
        return V(self.ap.bitcast(dt), self.tok)

    def re(self, s, **kw):
        return V(self.ap.rearrange(s, **kw), self.tok)

    def bcast(self, shape):
        return V(self.ap.to_broadcast(list(shape)), self.tok)

    def bto(self, shape):
        return V(self.ap.broadcast_to(list(shape)), self.tok)

    def sub(self, tok):
        return V(self.ap, tok)


class Prog:
    def __init__(self, nc, ndma=32):
        self.nc = nc
        self.es = ExitStack()
        self.scopes = []
        self.stream = {e: [] for e in ENGS}
        self.know = {e: {} for e in ENGS}
        self.tok = {}
        self.esem = {e: self.es.enter_context(nc.semaphore("s_" + e)) for e in ENGS}
        self.ndma = ndma
        self.dsem = [self.es.enter_context(nc.semaphore("d%d" % i)) for i in range(ndma + 1)]
        self.dcnt = [0] * (ndma + 1)
        self.dsnap = {}
        self.dn = 0
        self.uid = 0
        self.dry = False

    def push(self):
        self.scopes.append(ExitStack())

    def pop(self):
        self.scopes.pop().close()
        self.barrier()

    def _ctx(self):
        return self.scopes[-1] if self.scopes else self.es

    def sb(self, name, shape, dt=F32):
        self.uid += 1
        nm = "%s_%d" % (name, self.uid)
        t = self._ctx().enter_context(self.nc.sbuf_tensor(nm, list(shape), dt))
        return V(t[:], nm)

    def ps(self, name, shape, dt=F32):
        t = self.es.enter_context(self.nc.psum_tensor(name, list(shape), dt))
        return V(t[:], name)

    def _deps(self, reads, writes):
        deps = set()
        for t in reads:
            st = self.tok.get(t)
            if st and st[0] is not None:
                deps.add(st[0])
        for t in writes:
            st = self.tok.get(t)
            if st:
                if st[0] is not None:
                    deps.add(st[0])
                deps.update(st[1])
        return deps

    def _snap_of(self, ev):
        if ev[0] == "e":
            return self.stream[ev[1]][ev[2]]["snap"]
        return self.dsnap[(ev[1], ev[2])]

    def _resolve(self, eng, deps, is_dma):
        kn = self.know[eng]
        best = {}
        for ev in deps:
            key = (ev[0], ev[1])
            if best.get(key, (-1,))[0] < ev[2]:
                best[key] = (ev[2], ev)
        waits = []
        for key, (val, ev) in best.items():
            if key == ("e", eng) and eng == "pe":
                continue
            if kn.get(key, -1) >= val:
                continue
            if ev[0] == "e":
                self.stream[ev[1]][ev[2]]["inc"] = True
            waits.append(ev)
            snap = self._snap_of(ev)
            for k2, v2 in snap.items():
                if kn.get(k2, -1) < v2:
                    kn[k2] = v2
            if kn.get(key, -1) < val:
                kn[key] = val
        return waits

    def _register(self, ev, reads, writes):
        for t in reads:
            st = self.tok.get(t)
            if st is None:
                st = [None, []]
                self.tok[t] = st
            st[1].append(ev)
        for t in writes:
            self.tok[t] = [ev, []]

    def op(self, eng, fn, reads=(), writes=()):
        if self.dry:
            return None
        deps = self._deps(reads, writes)
        waits = self._resolve(eng, deps, False)
        seq = len(self.stream[eng])
        snap = dict(self.know[eng])
        snap[("e", eng)] = seq
        self.stream[eng].append({"fn": fn, "waits": waits, "inc": False, "dma": None, "snap": snap})
        ev = ("e", eng, seq)
        self._register(ev, reads, writes)
        return ev

    def dma(self, q, out, in_, reads=(), writes=(), **kw):
        if self.dry:
            return None
        reads, writes = [in_.tok] + list(reads), [out.tok] + list(writes)
        k = self.dn % self.ndma
        self.dn += 1
        deps = self._deps(reads, writes)
        if self.dcnt[k] > 0:
            deps.add(("d", k, self.dcnt[k]))
        waits = self._resolve(q, deps, True)
        self.dcnt[k] += 16
        val = self.dcnt[k]

        def fn(eng, o=out.ap, i=in_.ap, kw=kw):
            return eng.dma_start(out=o, in_=i, **kw)

        self.stream[q].append({"fn": fn, "waits": waits, "inc": False, "dma": k,
                               "snap": dict(self.know[q])})
        self.dsnap[(k, val)] = dict(self.know[q])
        ev = ("d", k, val)
        self._register(ev, reads, writes)
        return ev

    def barrier(self):
        last = {}
        for e in ENGS:
            st = self.stream[e]
            for i in range(len(st) - 1, -1, -1):
                if st[i]["fn"] is not None and st[i]["dma"] is None:
                    last[e] = ("e", e, i)
                    break
        deps = set(last.values())
        for k in range(self.ndma + 1):
            if self.dcnt[k] > 0:
                deps.add(("d", k, self.dcnt[k]))
        for e in ENGS:
            waits = self._resolve(e, deps, True)
            self.stream[e].append({"fn": None, "waits": waits, "inc": False, "dma": None,
                                   "snap": dict(self.know[e])})
        self.tok = {}

    def emit(self):
        nc = self.nc
        pref = {}
        for e in ENGS:
            c = 0
            arr = []
            for ent in self.stream[e]:
                if ent["inc"]:
                    c += 1
                arr.append(c)
            pref[e] = arr

        def replay(e, eng):
            for ent in self.stream[e]:
                for ev in ent["waits"]:
                    if ev[0] == "e":
                        eng.wait_ge(self.esem[ev[1]], pref[ev[1]][ev[2]])
                    else:
                        eng.wait_ge(self.dsem[ev[1]], ev[2])
                if ent["fn"] is None:
                    continue
                ins = ent["fn"](eng)
                if ent["dma"] is not None:
                    ins.then_inc(self.dsem[ent["dma"]], ent.get("dinc", 16))
                elif ent["inc"]:
                    ins.then_inc(self.esem[e], 1)

        with nc.Block() as block:
            @block.tensor
            def _(eng):
                replay("pe", eng)

            @block.scalar
            def _(eng):
                replay("act", eng)

            @block.vector
            def _(eng):
                replay("dve", eng)

            @block.gpsimd
            def _(eng):
                replay("pool", eng)

            @block.sync
            def _(eng):
                replay("sp", eng)

    def close(self):
        while self.scopes:
            self.pop()
        self.es.close()

    @staticmethod
    def _rw(outs, ins):
        w = [o.tok for o in outs if isinstance(o, V)]
        r = [i.tok for i in ins if isinstance(i, V)]
        return r, w

    @staticmethod
    def _a(x):
        return x.ap if isinstance(x, V) else x

    def mm(self, out, lhsT, rhs, start=True, stop=True):
        r, w = self._rw([out], [lhsT, rhs])
        if not start:
            r = r + [out.tok]
        self.op("pe", lambda e, o=out.ap, l=lhsT.ap, rr=rhs.ap: e.matmul(o, lhsT=l, rhs=rr, start=start, stop=stop), r, w)

    def tr(self, out, in_, ident):
        r, w = self._rw([out], [in_, ident])
        self.op("pe", lambda e, o=out.ap, i=in_.ap, d=ident.ap: e.transpose(out=o, in_=i, identity=d), r, w)

    def act(self, out, in_, func, scale=None, bias=None, accum=None, extra_w=()):
        r, w = self._rw([out, accum], [in_, scale, bias])
        kw = {}
        if scale is not None:
            kw["scale"] = self._a(scale)
        if bias is not None:
            kw["bias"] = self._a(bias)
        if accum is not None:
            kw["accum_out"] = accum.ap
        self.op("act", lambda e, o=out.ap, i=in_.ap, kw=kw: e.activation(out=o, in_=i, func=func, **kw), r, w)

    def tt(self, eng, out, in0, in1, op):
        r, w = self._rw([out], [in0, in1])
        self.op(eng, lambda e, o=out.ap, a=in0.ap, b=in1.ap: e.tensor_tensor(out=o, in0=a, in1=b, op=op), r, w)

    def ts(self, eng, out, in0, s1, s2, op0, op1=None):
        r, w = self._rw([out], [in0, s1, s2])
        a1, a2 = self._a(s1), self._a(s2)
        if op1 is None:
            self.op(eng, lambda e, o=out.ap, a=in0.ap: e.tensor_scalar(out=o, in0=a, scalar1=a1, scalar2=None, op0=op0), r, w)
        else:
            self.op(eng, lambda e, o=out.ap, a=in0.ap: e.tensor_scalar(out=o, in0=a, scalar1=a1, scalar2=a2, op0=op0, op1=op1), r, w)

    def stt(self, out, in0, scalar, in1, op0, op1):
        r, w = self._rw([out], [in0, scalar, in1])
        s = self._a(scalar)
        self.op("dve", lambda e, o=out.ap, a=in0.ap, b=in1.ap: e.scalar_tensor_tensor(out=o, in0=a, scalar=s, in1=b, op0=op0, op1=op1), r, w)

    def copy(self, eng, out, in_):
        r, w = self._rw([out], [in_])
        if eng == "act":
            self.op("act", lambda e, o=out.ap, i=in_.ap: e.copy(out=o, in_=i), r, w)
        else:
            self.op(eng, lambda e, o=out.ap, i=in_.ap: e.tensor_copy(out=o, in_=i), r, w)

    def memset(self, eng, out, val):
        r, w = self._rw([out], [])
        self.op(eng, lambda e, o=out.ap: e.memset(o, val), r, w)

    def reduce(self, out, in_, op, axis=AX.X):
        r, w = self._rw([out], [in_])
        self.op("dve", lambda e, o=out.ap, i=in_.ap: e.tensor_reduce(out=o, in_=i, axis=axis, op=op), r, w)

    def recip(self, out, in_):
        r, w = self._rw([out], [in_])
        self.op("dve", lambda e, o=out.ap, i=in_.ap: e.reciprocal(out=o, in_=i), r, w)

    def scan(self, out, d0, d1, init, op0, op1):
        r, w = self._rw([out], [d0, d1])
        self.op("dve", lambda e, o=out.ap, a=d0.ap, b=d1.ap: e.tensor_tensor_scan(out=o, data0=a, data1=b, initial=init, op0=op0, op1=op1), r, w)


IN_OFF = dict(hq=0, hf=512, hi=1024, hgate=1536, cq=2048, ckv=2432, kr=2688, ga=2720, gb=3744)


def _blk(w):
    K, n = w.shape
    kc = K // 128
    return np.ascontiguousarray(w.reshape(kc, 128, n).transpose(1, 0, 2)).reshape(128, kc * n)


def _pad(a, width=1024, rows=128):
    out = np.zeros((rows, width), np.float32)
    out[: a.shape[0], : a.shape[1]] = a
    return out


S1_BLOCKS = ([("hq", h) for h in range(4)] + [("hgate", h) for h in range(4)] + [("gb", j) for j in range(8)]
             + [("hf", h) for h in range(4)] + [("hi", h) for h in range(4)]
             + [("cq", j) for j in range(3)] + [("ckv", j) for j in range(2)] + [("kr", 0), ("krs", 0)]
             + [x for h in range(8) for x in (("wq", h), ("wqs", h), ("wk", h))] + [("wv", 0)]
             + [x for j in range(8) for x in (("ga", j), ("bra", j))])
S1_IDX = {b: i for i, b in enumerate(S1_BLOCKS)}


def prep_weights(inp):
    f = lambda a: np.asarray(a, dtype=np.float32)
    w_in, w_q_up, w_kv_up = f(inp["w_in"]), f(inp["w_q_up"]), f(inp["w_kv_up"])
    w_br_a, w_br_b, w_out = f(inp["w_br_a"]), f(inp["w_br_b"]), f(inp["w_out"])
    w_gate, w_up, w_down = f(inp["w_gate"]), f(inp["w_up"]), f(inp["w_down"])
    ada_w = f(inp["ada_w"])
    sw = np.concatenate([np.arange(16, 32), np.arange(0, 16)])
    w1 = np.zeros((DEPTH, len(S1_BLOCKS), 128, 1024), np.float32)
    w3b = np.zeros((DEPTH, 128, 4 * 1024), np.float32)
    w3a = np.zeros((DEPTH, 128, 4 * 1024), np.float32)
    w3o = np.zeros((DEPTH, 8, 128, 1024), np.float32)
    w4 = np.zeros((DEPTH, NEXP, 12, 128, 1024), np.float32)
    adaw = np.zeros((DEPTH, 48, 128, 1024), np.float32)
    for l in range(DEPTH):
        for bi, (kind, j) in enumerate(S1_BLOCKS):
            if kind in ("hq", "hf", "hi", "hgate", "cq", "ckv", "ga", "gb"):
                c0 = IN_OFF[kind] + 128 * j
                w1[l, bi] = _blk(w_in[l][:, c0:c0 + 128])
            elif kind in ("kr", "krs"):
                kr = w_in[l][:, 2688:2720]
                if kind == "krs":
                    kr = kr[:, sw]
                m = np.zeros((1024, 128), np.float32)
                m[:, 64:96] = kr
                w1[l, bi] = _blk(m)
            elif kind in ("wq", "wqs"):
                q = w_q_up[l][:, j * 96:(j + 1) * 96]
                m = np.zeros((384, 128), np.float32)
                if kind == "wq":
                    m[:, 0:96] = q
                else:
                    m[:, 64:96] = q[:, 64:96][:, sw]
                w1[l, bi] = _pad(_blk(m))
            elif kind == "wk":
                m = np.zeros((256, 128), np.float32)
                m[:, 0:64] = w_kv_up[l][:, j * 128:j * 128 + 64]
                w1[l, bi] = _pad(_blk(m))
            elif kind == "wv":
                m = np.zeros((256, 512), np.float32)
                for h in range(8):
                    m[:, h * 64:(h + 1) * 64] = w_kv_up[l][:, h * 128 + 64:h * 128 + 128]
                w1[l, bi] = _blk(m)
            elif kind == "bra":
                w1[l, bi] = _pad(_blk(w_br_a[l][:, j * 128:(j + 1) * 128]))
        w3b[l] = _blk(w_br_b[l])
        w3a[l] = _blk(w_br_a[l])
        for j in range(8):
            w3o[l, j] = w_out[l][j * 128:(j + 1) * 128, :]
        for e in range(NEXP):
            for j in range(4):
                w4[l, e, 2 * j] = _blk(w_gate[l, e][:, j * 128:(j + 1) * 128])
                w4[l, e, 2 * j + 1] = _blk(w_up[l, e][:, j * 128:(j + 1) * 128])
                w4[l, e, 8 + j] = w_down[l, e][j * 128:(j + 1) * 128, :]
        for cg in range(12):
            for q in range(4):
                adaw[l, cg * 4 + q] = _blk(ada_w[l][q * 256:(q + 1) * 256, cg * 512:(cg + 1) * 512])
    w1h = [w1, w1.copy()]
    for half in range(2):
        for l in range(DEPTH):
            for j in range(4):
                gh = half * 4 + j
                q = w_q_up[l][:, gh * 96:(gh + 1) * 96]
                m = np.zeros((384, 128), np.float32)
                m[:, 0:96] = q
                w1h[half][l, S1_IDX[("wq", j)]] = _pad(_blk(m))
                m = np.zeros((384, 128), np.float32)
                m[:, 64:96] = q[:, 64:96][:, sw]
                w1h[half][l, S1_IDX[("wqs", j)]] = _pad(_blk(m))
                m = np.zeros((256, 128), np.float32)
                m[:, 0:64] = w_kv_up[l][:, gh * 128:gh * 128 + 64]
                w1h[half][l, S1_IDX[("wk", j)]] = _pad(_blk(m))
            m = np.zeros((256, 512), np.float32)
            for j in range(4):
                gh = half * 4 + j
                m[:, j * 64:(j + 1) * 64] = w_kv_up[l][:, gh * 128 + 64:gh * 128 + 128]
            w1h[half][l, S1_IDX[("wv", 0)]] = _blk(m)
            for j in range(2):
                gh = half * 2 + j
                for kind in ("hq", "hf", "hi", "hgate"):
                    c0 = IN_OFF[kind] + 128 * gh
                    w1h[half][l, S1_IDX[(kind, j)]] = _blk(w_in[l][:, c0:c0 + 128])
    pv = lambda a, n: np.ascontiguousarray(f(a).reshape(DEPTH, n, 128).transpose(2, 0, 1))
    cstf = np.zeros((128, 1024), np.float32)
    cstf[:, 0:128] = np.eye(128, dtype=np.float32)
    cstf[0:32, 128:160] = np.triu(np.ones((32, 32), np.float32))
    m = np.ones(512, np.float32)
    m[0::32] = 0.0
    cstf[:, 160:672] = m[None, :]
    inv = (np.float32(ROPE_BASE) ** (-np.arange(16, dtype=np.float32) / np.float32(16))).astype(np.float32)
    cstf[64:96, 672] = np.concatenate([inv, inv])
    cstf[64:80, 673] = -1.0
    cstf[80:96, 673] = 1.0
    cstr = np.zeros((128, 256), np.float32)
    cstr[:, 128:256] = 1.0
    return dict(
        w1=w1h, w3b=w3b, w3a=w3a, w3o=w3o, w4=w4, adaw=adaw, cstf=cstf, cstr=cstr,
        adab=f(inp["ada_b"]).reshape(DEPTH, 1, 6 * D),
        n1g=f(inp["norm1_g"]).reshape(DEPTH, 1, D), n2g=f(inp["norm2_g"]).reshape(DEPTH, 1, D),
        fing=f(inp["final_g"]).reshape(1, D),
        lbl=[np.ascontiguousarray(pv(inp["hg_lb_logits"], 4)[:, :, [2 * hf_, 2 * hf_ + 1, 2 * hf_, 2 * hf_ + 1]]) for hf_ in range(2)],
        hgn=[np.ascontiguousarray(pv(inp["hg_norm_g"], 4)[:, :, [2 * hf_, 2 * hf_ + 1, 2 * hf_, 2 * hf_ + 1]]) for hf_ in range(2)],
        qng=pv(inp["q_norm_g"], 3), kvng=pv(inp["kv_norm_g"], 2),
        wrt=_blk(f(inp["w_router"])), rbias=f(inp["router_bias"]).reshape(1, NEXP),
    )


def build(T, nlayers=DEPTH, upto="all", dbg=False):
    nc = bass.Bass("TRN2", target_bir_lowering=False)
    nc.dge_precook = False
    NT = T // 512
    NKC = T // 128
    P = Prog(nc)

    def din(name, shape, dt=F32):
        return V(nc.dram_tensor(name, list(shape), dt, kind="ExternalInput").ap(), name)

    def dscr(name, shape, dt=F32, out=False):
        kind = "ExternalOutput" if (out or dbg) else "Internal"
        return V(nc.dram_tensor(name, list(shape), dt, kind=kind).ap(), name)

    x_in = din("x", [T, D])
    cvec = din("cvec", [128, 8])
    pos = din("pos", [1, T], I32)
    w1 = din("w1", [DEPTH, len(S1_BLOCKS), 128, 1024], F32R)
    w3b = din("w3b", [DEPTH, 128, 4 * 1024], F32R)
    w3a = din("w3a", [DEPTH, 128, 4 * 1024], F32R)
    w3o = din("w3o", [DEPTH, 8, 128, 1024], F32R)
    w4 = din("w4", [DEPTH, NEXP, 12, 128, 1024], F32R)
    adaw = din("adaw", [DEPTH, 48, 128, 1024], F32R)
    cstf_d = din("cstf", [128, 1024])
    cstr_d = din("cstr", [128, 256], F32R)
    adab = din("adab", [DEPTH, 1, 6 * D])
    n1g = din("n1g", [DEPTH, 1, D])
    n2g = din("n2g", [DEPTH, 1, D])
    fing = din("fing", [1, D])
    lbl_d = din("lbl", [128, DEPTH, 4])
    hgn_d = din("hgn", [128, DEPTH, 4])
    qng_d = din("qng", [128, DEPTH, 3])
    kvng_d = din("kvng", [128, DEPTH, 2])
    wrt_d = din("wrt", [128, 8 * NEXP], F32R)
    rbias_d = din("rbias", [1, NEXP])
    hsel_d = din("hsel", [128, 2], I32)

    TH = T // 2
    out_d = dscr("out", [TH, D], out=True)
    xa = dscr("xa", [T, D])
    CR = 512
    NCH = TH // CR
    xg = [dscr("xg%d" % c, [2 * CR, D]) for c in range(NCH)]
    xh_c = [dscr("xh%d" % c, [CR, D]) for c in range(NCH)]

    def xblk(l, bi):
        if l == 0:
            return x_in[bi * 128:(bi + 1) * 128, :].sub(("x", bi))
        h, wr = divmod(bi * 128, TH)
        c, r = divmod(wr, CR)
        return xg[c][h * CR + r:h * CR + r + 128, :].sub(("xb", bi))
    modd = dscr("modd", [DEPTH, 1, 6 * D])
    rot = dscr("rot", [2, 32, T])
    ma_d = dscr("ma", [D, T])
    gb_d = dscr("gb", [D, T])
    qT_d = dscr("qT", [MLA_H, 96, T], F32R)
    kT_d = dscr("kT", [MLA_H, 96, T], F32R)
    vA_d = dscr("vA", [MLA_H, 128, NKC, 65], F32R)
    NHL = MLA_H // 2
    NGL = HG_H // 2
    onm = [dscr("onm%d" % c, [128, T]) for c in range(NGL)]
    ong = [dscr("ong%d" % c, [256, T]) for c in range(NGL)]
    atm = [dscr("atm%d" % c, [128, T]) for c in range(2)]
    atg = [dscr("atg%d" % c, [256, T]) for c in range(2)]

    cstf = P.sb("cstf", [128, 1024])
    cstr = P.sb("cstr", [128, 256], F32R)
    ident = cstf[:, 0:128]
    tri = cstf[0:32, 128:160]
    scanmask = cstf[:, 160:672]
    inv_c = cstf[:, 672:673]
    sgn_c = cstf[:, 673:674]
    onesR = cstr[:, 128:256]
    lb = P.sb("lb", [128, DEPTH, 4])
    oml = P.sb("oml", [128, DEPTH, 4])
    hgn = P.sb("hgn", [128, DEPTH, 4])
    qng = P.sb("qng", [128, DEPTH, 3])
    kvng = P.sb("kvng", [128, DEPTH, 2])
    A_t = P.sb("A_t", [128, D])
    B_t = P.sb("B_t", [128, D])
    G_t = P.sb("G_t", [128, D])
    ring = []
    rcnt = [0]
    pb = [P.ps("pb%d" % i, [128, 512]) for i in range(8)]

    live = [1]

    def make_ring(n, nlive=1):
        live[0] = nlive
        del ring[:]
        ring.extend(P.sb("ring%d" % i, [128, 1024], F32R) for i in range(n))
        rcnt[0] = 0

    wplan = {"seq": [], "issued": 0, "used": 0}

    def plan_weights(fn):
        P.dry = True
        wplan["seq"], wplan["issued"], wplan["used"] = [], 0, 0
        fn()
        P.dry = False

    def pool_dma(fn_make, rd, wr, coll=False):
        if P.dry:
            return
        if coll:
            k, dinc = P.ndma, 1
        else:
            k, dinc = P.dn % P.ndma, 16
            P.dn += 1
        deps = P._deps(rd, wr)
        if P.dcnt[k] > 0:
            deps.add(("d", k, P.dcnt[k]))
        waits = P._resolve("pool", deps, True)
        P.dcnt[k] += dinc
        val = P.dcnt[k]
        P.stream["pool"].append({"fn": fn_make, "waits": waits, "inc": False, "dma": k, "dinc": dinc,
                                 "snap": dict(P.know["pool"])})
        P.dsnap[(k, val)] = dict(P.know["pool"])
        P._register(("d", k, val), rd, wr)

    def gather_rows(out, in_, idx, bound, reads=()):
        io = bass.IndirectOffsetOnAxis(ap=idx.ap, axis=0)
        pool_dma(lambda eng, o=out.ap, i=in_.ap: eng.indirect_dma_start(out=o, out_offset=None, in_=i, in_offset=io,
                                                                      bounds_check=bound, oob_is_err=False),
                 [idx.tok] + list(reads), [out.tok])

    def getw(src, width, parts=128):
        if P.dry:
            wplan["seq"].append((src, width, parts))
            return ring[0]
        i = wplan["used"]
        assert wplan["seq"][i][1] == width and wplan["seq"][i][2] == parts
        NR = len(ring)
        while wplan["issued"] < min(len(wplan["seq"]), i + NR - (live[0] - 1)):
            j = wplan["issued"]
            s_, w_, p_ = wplan["seq"][j]
            slot = ring[(rcnt[0] + j) % NR]
            P.dma("sp", slot[0:p_, 0:w_], s_[0:p_, 0:w_])
            wplan["issued"] += 1
        slot = ring[(rcnt[0] + i) % NR]
        wplan["used"] += 1
        if wplan["used"] == len(wplan["seq"]):
            rcnt[0] += len(wplan["seq"])
            wplan["seq"], wplan["issued"], wplan["used"] = [], 0, 0
        return slot

    P.dma("sp", cstf, cstf_d)
    P.dma("sp", cstr, cstr_d)
    P.dma("sp", hgn, hgn_d)
    P.dma("sp", qng, qng_d)
    P.dma("sp", kvng, kvng_d)

    eps_t = P.sb("eps_t", [128, 1])
    P.memset("dve", eps_t, EPS)

    P.push()
    lbl = P.sb("lbl", [128, DEPTH, 4])
    P.dma("sp", lbl, lbl_d)
    t4 = P.sb("t4", [128, 4])
    P.tt("dve", t4, lbl[:, 1, :], lbl[:, 0, :], ALU.subtract)
    P.memset("dve", lb[:, 0, :], 0.0)
    P.act(lb[:, 1, :], t4, AF.Sigmoid)
    P.ts("dve", oml, lb, -1.0, 1.0, ALU.mult, ALU.add)

    pi_ = P.sb("pi", [128, 512], I32)
    pf = P.sb("pf", [128, 512])
    ang = P.sb("ang", [128, 512])
    rr = [P.sb("rr", [128, 512]) for _ in range(2)]
    tb = [P.sb("tb", [128, 512]) for _ in range(2)]
    for n in range(NT):
        P.dma("sp", pi_[64:96, :], pos[:, n * 512:(n + 1) * 512].bto([32, 512]))
        P.copy("dve", pf[64:96, :], pi_[64:96, :])
        P.ts("dve", ang[64:96, :], pf[64:96, :], inv_c[64:96, :], None, ALU.mult)
        for which, shift in ((0, np.pi / 2), (1, 0.0)):
            r_, t_ = rr[which], tb[which]
            a2, kk_, mm_ = tb[which], pf, pi_.bc(F32)
            P.ts("dve", a2[64:96, :], ang[64:96, :], float(shift), None, ALU.add)
            P.ts("dve", kk_[64:96, :], a2[64:96, :], float(1.0 / TWO_PI), 12582912.0, ALU.mult, ALU.add)
            P.ts("dve", kk_[64:96, :], kk_[64:96, :], -12582912.0, None, ALU.add)
            P.stt(r_[64:96, :], kk_[64:96, :], -6.28125, a2[64:96, :], ALU.mult, ALU.add)
            P.stt(r_[64:96, :], kk_[64:96, :], float(-(TWO_PI - 6.28125)), r_[64:96, :], ALU.mult, ALU.add)
            P.ts("dve", mm_[64:96, :], r_[64:96, :], float(np.pi), float(-TWO_PI), ALU.is_gt, ALU.mult)
            P.tt("dve", r_[64:96, :], r_[64:96, :], mm_[64:96, :], ALU.add)
            P.ts("dve", mm_[64:96, :], r_[64:96, :], float(-np.pi), float(TWO_PI), ALU.is_lt, ALU.mult)
            P.tt("dve", r_[64:96, :], r_[64:96, :], mm_[64:96, :], ALU.add)
            P.ts("dve", r_[64:96, :], r_[64:96, :], float(-np.pi), float(np.pi), ALU.max, ALU.min)
            P.act(t_[64:96, :], r_[64:96, :], AF.Sin)
            if which == 1:
                P.ts("dve", t_[64:96, :], t_[64:96, :], sgn_c[64:96, :], None, ALU.mult)
            P.dma("sp", rot[which, :, n * 512:(n + 1) * 512].sub(("rot", n)), t_[64:96, :])
    P.pop()

    P.push()
    cv = P.sb("cv", [128, 8])
    cact = P.sb("cact", [128, 8], F32R)
    make_ring(6)
    P.dma("sp", cv, cvec)
    sg = P.sb("sg", [128, 8])
    P.act(sg, cv, AF.Sigmoid)
    P.tt("dve", cact, cv, sg, ALU.mult)
    ab = P.sb("ab", [1, 6 * D])
    mrow = P.sb("mrow", [1, 6 * D])

    def mod_layer(l):
        P.dma("sp", ab, adab[l])
        for cg in range(12):
            acc = pb[cg % 2]
            for q in range(4):
                slot = getw(adaw[l, cg * 4 + q], 1024)
                for k2 in range(2):
                    kc = q * 2 + k2
                    P.mm(acc[0:1, :], cact[:, kc:kc + 1], slot[:, k2 * 512:(k2 + 1) * 512],
                         start=(kc == 0), stop=(kc == 7))
            P.tt("dve", mrow[:, cg * 512:(cg + 1) * 512], acc[0:1, :], ab[:, cg * 512:(cg + 1) * 512], ALU.add)
        P.dma("sp", modd[l].sub(("modd", l)), mrow)

    for l in range(nlayers):
        plan_weights(lambda: mod_layer(l))
        mod_layer(l)
    P.pop()

    def load_mod(l, which):
        ng = n1g if which == 0 else n2g
        o = 3 * D * which
        md = modd[l].sub(("modd", l))
        P.dma("sp", B_t, md[:, o:o + D].bto([128, D]))
        P.dma("sp", A_t, md[:, o + D:o + 2 * D].bto([128, D]))
        P.dma("sp", G_t, md[:, o + 2 * D:o + 3 * D].bto([128, D]))
        P.push()
        gt = P.sb("gt", [128, D])
        P.dma("sp", gt, ng[l].bto([128, D]))
        P.stt(A_t, A_t, 1.0, gt, ALU.add, ALU.mult)
        P.pop()

    def norm_mod(xt, h_out, junk, ss):
        P.act(junk, xt, AF.Square, accum=ss)
        P.act(ss, ss, AF.Ln, scale=1.0 / D, bias=eps_t)
        P.act(ss, ss, AF.Exp, scale=-0.5)
        P.stt(h_out, xt, ss, A_t, ALU.mult, ALU.mult)
        P.tt("dve", h_out, h_out, B_t, ALU.add)

    R = lambda v: v.bc(F32R)
    nbc = [0]

    def nb():
        b = pb[nbc[0] % 3]
        nbc[0] += 1
        return b

    QSCALE = 0.5 * 128 ** -0.5

    def stage1(l):
        load_mod(l, 0)
        P.push()
        make_ring(6)
        xt = [P.sb("xt", [128, D]) for _ in range(2)]
        hh = [P.sb("hh", [128, D]) for _ in range(2)]
        junk = P.sb("junk", [128, D])
        ss = P.sb("ss", [128, 1])
        hT = P.sb("hT", [128, 8, 512])
        tl = lambda nm: P.sb(nm, [128, 512])
        qs = [tl("qs") for _ in range(4)]
        sgate = [tl("sgate") for _ in range(4)]
        tt_ = tl("tt_")
        gbt = [tl("gbt") for _ in range(2)]
        hgA = P.sb("hgA", [128, 9, 512])
        hgR = P.sb("hgR", [128, 8, 512])
        aslot = lambda i: hgA[:, i, :].sub(("hgA", i))
        rslot = lambda i: hgR[:, i, :].sub(("hgR", i))
        t1, t2, lf, kk, bb, eb, enb, kd, vt = [aslot(i) for i in range(9)]
        kh, qh = rslot(0), rslot(1)
        o_t, sq, t_l, t_on, tmpA, tmpB, kpe, Ct, St = [tl("m%d" % i) for i in range(9)]
        kdT = P.sb("kdT", [32, 16, 128])
        vT = P.sb("vT", [32, 16, 128])
        aT = P.sb("aT", [32, 16, 32])
        Swork = P.sb("Swork", [128, 17, 128])
        Scar = P.sb("Scar", [128, 4, 128])
        ebl = P.sb("ebl", [128, 16])
        onT = P.sb("onT", [128, 4, 512])
        cq = [aslot(i) for i in range(3)]
        sqq = [rslot(2 + i) for i in range(3)]
        cqn = [rslot(5 + i) for i in range(3)]
        ckvn = [rslot(4), rslot(3)]
        qtb = [tl("qt") for _ in range(2)]
        ktb = [tl("kt") for _ in range(2)]
        va = P.sb("va", [128, 8, 4, 65])
        gat = gbt
        mat = [o_t, t_on]
        tri3 = tri.re("p (o t) -> p o t", o=1).bcast([32, 16, 32])
        P.memset("dve", Scar, 0.0)
        P.memset("dve", va, 1.0)
        Sw = lambda c: Swork[:, c, :].sub(("Sw", c))

        def proj(kind, j):
            bank = nb()
            slot = getw(w1[l, S1_IDX[(kind, j)]], 1024)
            for kc in range(8):
                P.mm(bank, slot[:, kc * 128:(kc + 1) * 128], R(hT[:, kc, :]), start=(kc == 0), stop=(kc == 7))
            return bank

        def core(h, mid=None):
            for src_t, dst in ((kd, kdT), (vt, vT)):
                for g in range(4):
                    bank = nb()
                    for c4 in range(4):
                        c = g * 4 + c4
                        P.tr(bank[0:32, c4 * 128:(c4 + 1) * 128], src_t[:, c * 32:(c + 1) * 32], ident)
                    P.copy("act" if g % 2 else "dve", R(dst[:, g * 4:(g + 1) * 4, :]),
                           bank[0:32, :].re("p (c d) -> p c d", d=128))
            for c in range(16):
                P.mm(pb[3][0:32, c * 32:(c + 1) * 32], R(kh[:, c * 32:(c + 1) * 32]), R(qh[:, c * 32:(c + 1) * 32]))
            P.tt("dve", R(aT), pb[3][0:32, :].re("p (c t) -> p c t", t=32), tri3, ALU.mult)
            P.copy("dve", R(Sw(0)), Scar[:, h, :])
            for g in range(4):
                ubk = pb[4 + g % 2]
                for c4 in range(4):
                    c = g * 4 + c4
                    P.mm(ubk[:, c4 * 128:(c4 + 1) * 128], R(kdT[:, c, :]), R(vT[:, c, :]))
                for c4 in range(4):
                    c = g * 4 + c4
                    P.stt(R(Sw(c + 1)), Sw(c), ebl[:, c:c + 1], ubk[:, c4 * 128:(c4 + 1) * 128], ALU.mult, ALU.add)
            P.copy("dve", Scar[:, h, :], Sw(16))
            if mid is not None:
                mid()
            for c in range(16):
                ob = pb[6][:, c * 32:(c + 1) * 32]
                P.mm(ob, R(vT[:, c, :]), R(aT[:, c, :]), start=True, stop=False)
                P.mm(ob, R(Sw(c)), R(qh[:, c * 32:(c + 1) * 32]), start=False, stop=True)
            P.copy("act", o_t, pb[6])
            P.act(R(sq), pb[6], AF.Square)
            P.mm(pb[7], onesR, R(sq))
            P.act(t_l, pb[7], AF.Ln, scale=1.0 / 128, bias=eps_t)
            P.act(t_l, t_l, AF.Exp, scale=-0.5)
            P.stt(t_on, o_t, hgn[:, l, h:h + 1], t_l, ALU.mult, ALU.mult)
            P.stt(R(onT[:, h, :]), sgate[h], 0.5, t_on, ALU.mult, ALU.mult)
            P.dma("sp", onm[h][:, cur["cols"]].sub(("on", h, cur["n"])), onT[:, h, :])

        cur = {}

        def tile(n):
            cols = slice(n * 512, (n + 1) * 512)
            cur["cols"], cur["n"] = cols, n
            P.dma("sp", Ct[64:96, :], rot[0, :, cols].sub(("rot", n)))
            P.dma("sp", St[64:96, :], rot[1, :, cols].sub(("rot", n)))
            for s in range(4):
                r0 = (n * 4 + s) * 128
                P.dma("sp", xt[s % 2], xblk(l, n * 4 + s))
                norm_mod(xt[s % 2], hh[s % 2], junk, ss)
                for half in range(2):
                    bank = nb()
                    for k4 in range(4):
                        kc = half * 4 + k4
                        P.tr(bank[:, k4 * 128:(k4 + 1) * 128], hh[s % 2][:, kc * 128:(kc + 1) * 128], ident)
                    P.copy("act" if half else "dve", R(hT[:, half * 4:(half + 1) * 4, s * 128:(s + 1) * 128]),
                           bank.re("p (k t) -> p k t", t=128))
            def mla_latents():
                for name, nj, gv, dst in (("cq", 3, qng, cqn), ("ckv", 2, kvng, ckvn)):
                    for j in range(nj):
                        bank = proj(name, j)
                        P.copy("act", cq[j], bank)
                        P.act(R(sqq[j]), bank, AF.Square)
                    for j in range(nj):
                        P.mm(pb[7], onesR, R(sqq[j]), start=(j == 0), stop=(j == nj - 1))
                    P.act(t_l, pb[7], AF.Ln, scale=1.0 / (128 * nj), bias=eps_t)
                    P.act(t_l, t_l, AF.Exp, scale=-0.5)
                    for j in range(nj):
                        P.stt(R(dst[j]), cq[j], gv[:, l, j:j + 1], t_l, ALU.mult, ALU.mult)
                b1 = proj("kr", 0)
                b2 = proj("krs", 0)
                P.tt("dve", kpe[64:96, :], b1[64:96, :], Ct[64:96, :], ALU.mult)
                P.tt("dve", tmpA[64:96, :], b2[64:96, :], St[64:96, :], ALU.mult)
                P.tt("dve", kpe[64:96, :], kpe[64:96, :], tmpA[64:96, :], ALU.add)

            def mla_head(h):
                qt, kt = qtb[h % 2], ktb[h % 2]
                bq, bq2 = nb(), nb()
                for kind, bank in (("wq", bq), ("wqs", bq2)):
                    slot = getw(w1[l, S1_IDX[(kind, h)]], 384)
                    for kc in range(3):
                        P.mm(bank, slot[:, kc * 128:(kc + 1) * 128], R(cqn[kc]), start=(kc == 0), stop=(kc == 2))
                P.copy("act", R(qt[0:64, :]), bq[0:64, :])
                P.tt("dve", tmpA[64:96, :], bq[64:96, :], Ct[64:96, :], ALU.mult)
                P.tt("dve", tmpB[64:96, :], bq2[64:96, :], St[64:96, :], ALU.mult)
                P.tt("dve", R(qt[64:96, :]), tmpA[64:96, :], tmpB[64:96, :], ALU.add)
                P.dma("sp", qT_d[h, :, cols].bc(F32).sub(("qT", h, n)), qt[0:96, :])
                bk = nb()
                slot = getw(w1[l, S1_IDX[("wk", h)]], 256)
                for kc in range(2):
                    P.mm(bk, slot[:, kc * 128:(kc + 1) * 128], R(ckvn[kc]), start=(kc == 0), stop=(kc == 1))
                P.copy("act", R(kt[0:64, :]), bk[0:64, :])
                P.copy("pool", R(kt[64:96, :]), kpe[64:96, :])
                P.dma("sp", kT_d[h, :, cols].bc(F32).sub(("kT", h)), kt[0:96, :])

            def mla_v():
                slot = getw(w1[l, S1_IDX[("wv", 0)]], 1024)
                for s in range(4):
                    bv = nb()
                    for kc in range(2):
                        P.mm(bv[:, 0:NHL * 64], R(ckvn[kc][:, s * 128:(s + 1) * 128]), slot[:, kc * 512:kc * 512 + NHL * 64],
                             start=(kc == 0), stop=(kc == 1))
                    P.copy("act", R(va[:, 0:NHL, s, 0:64]), bv[:, 0:NHL * 64].re("p (h v) -> p h v", v=64))
                for h in range(NHL):
                    P.dma("sp", vA_d[h, :, n * 4:(n + 1) * 4, :].bc(F32).sub(("vA", h)), va[:, h, :, :])

            mla_latents()
            for h in range(NGL):
                bank = proj("hq", h)
                P.act(tt_, bank, AF.Tanh, scale=0.5)
                P.stt(qs[h], tt_, 1.0, bank, ALU.add, ALU.mult)
            for h in range(NGL):
                bank = proj("hgate", h)
                P.act(tt_, bank, AF.Tanh, scale=0.5)
                P.stt(sgate[h], tt_, 1.0, bank, ALU.add, ALU.mult)
            for j in range(8):
                bank = proj("gb", j)
                P.act(tt_, bank, AF.Tanh, scale=0.5)
                P.ts("dve", gbt[j % 2], tt_, 0.5, 0.5, ALU.mult, ALU.add)
                P.dma("sp", gb_d[j * 128:(j + 1) * 128, cols].sub(("gb", j, n)), gbt[j % 2])
            for h in range(NGL):
                bank = proj("hf", h)
                bank_hi = proj("hi", h)
                P.act(t1, bank, AF.Exp, scale=-1.0)
                P.act(t1, t1, AF.Ln, bias=1.0)
                P.act(t2, t1, AF.Exp, scale=-1.0)
                P.ts("dve", t2, t2, oml[:, l, h:h + 1], lb[:, l, h:h + 1], ALU.mult, ALU.add)
                P.act(lf, t2, AF.Ln)
                P.ts("dve", kk, t2, -1.0, 1.0, ALU.mult, ALU.add)
                P.scan(bb, scanmask, lf, 0.0, ALU.mult, ALU.add)
                P.act(eb, bb, AF.Exp)
                P.act(enb, bb, AF.Exp, scale=-1.0)
                P.tt("dve", R(kh), kk, enb, ALU.mult)
                b3 = bb.re("p (c t) -> p c t", t=32)
                P.act(ebl, b3[:, :, 31], AF.Exp)
                P.tt("dve", kd.re("p (c t) -> p c t", t=32), kh.re("p (c t) -> p c t", t=32),
                     ebl.re("p (c o) -> p c o", o=1).bcast([128, 16, 32]), ALU.mult)
                P.stt(R(qh), qs[h], QSCALE, eb, ALU.mult, ALU.mult)
                P.copy("act", vt, bank_hi)
                mla_head(2 * h)
                core(h, mid=lambda h=h: mla_head(2 * h + 1))
            mla_v()
            for j in range(8):
                bank = proj("ga", j)
                P.act(tt_, bank, AF.Tanh, scale=0.5)
                P.ts("dve", gat[j % 2], tt_, 0.5, 0.5, ALU.mult, ALU.add)
                P.dma("sp", ma_d[j * 128:(j + 1) * 128, cols].sub(("ma", j, n)), gat[j % 2])

        def all_tiles():
            for n in range(NT):
                tile(n)

        plan_weights(all_tiles)
        all_tiles()
        for c in range(NGL):
            pool_dma(lambda eng, c=c: eng.collective_compute(
                "AllGather", ALU.bypass, replica_groups=[[0, 1], [2, 3], [4, 5], [6, 7]],
                ins=[onm[c].ap], outs=[ong[c].ap]), [("on", c, n2) for n2 in range(NT)], [("ong", c)], coll=True)
        P.pop()

    def stage2(l):
        P.push()
        SC = float(96 ** -0.5)
        ktl = [P.sb("ktl", [96, T], F32R) for _ in range(2)]
        val = [P.sb("val", [128, NKC, 65], F32R) for _ in range(2)]
        qtl = [P.sb("qtl", [96, 512], F32R) for _ in range(2)]
        pTs = [P.sb("pT", [128, 512]) for _ in range(4)]
        osb = [P.sb("osb", [64, 512]) for _ in range(2)]
        rdn = [P.sb("rdn", [128, 512]) for _ in range(2)]
        rdr = [P.sb("rdr", [128, 512]) for _ in range(2)]
        zt = P.sb("zt", [128, 512])
        P.memset("pool", zt, 0.0)
        ato = [P.sb("ato", [64, 512]) for _ in range(2)]
        cnt = 0
        pending = []

        def norm_tail(h, n, po):
            o_, r_, a_ = osb[n % 2], rdn[n % 2], ato[n % 2]
            P.copy("act", o_, po[0:64, :])
            P.recip(r_[64:65, :], po[64:65, :])
            r2_ = rdr[n % 2]
            P.copy("dve", R(r2_[64:65, :]), r_[64:65, :])
            pbx = pb[6 + n % 2]
            P.mm(pbx[0:64, :], onesR[64:65, 0:64], R(r2_[64:65, :]))
            P.tt("dve", R(a_), o_, pbx[0:64, :], ALU.mult)
            c = h // 2
            P.dma("sp", atm[c][(h % 2) * 64:(h % 2 + 1) * 64, n * 512:(n + 1) * 512].sub(("at", c, n)), a_)
            if h % 2 == 1 and n == NT - 1:
                pool_dma(lambda eng, c=c: eng.collective_compute(
                    "AllGather", ALU.bypass, replica_groups=[[0, 1], [2, 3], [4, 5], [6, 7]],
                    ins=[atm[c].ap], outs=[atg[c].ap]), [("at", c, n2) for n2 in range(NT)], [("atg", c)], coll=True)

        for h in range(NHL):
            kt_, va_ = ktl[h % 2], val[h % 2]
            P.dma("sp", kt_, kT_d[h].sub(("kT", h)))
            P.dma("sp", va_, vA_d[h].sub(("vA", h)))
            for n in range(NT):
                qt_ = qtl[n % 2]
                P.dma("sp", qt_, qT_d[h, :, n * 512:(n + 1) * 512].sub(("qT", h, n)))
                po = pb[4 + (n % 2)]
                nk = 4 * n + 4
                AHEAD = 2

                def score(j):
                    c0 = max(j - 4 * n, 0) * 128
                    ps = pb[(cnt + j) % 4]
                    P.mm(ps[:, c0:512], kt_[:, j * 128:(j + 1) * 128], qt_[:, c0:512])

                for j in range(min(AHEAD, nk)):
                    score(j)
                for j in range(nk):
                    jj = j - 4 * n
                    c0 = max(jj, 0) * 128
                    ps = pb[(cnt + j) % 4]
                    pt = pTs[(cnt + j) % 4]
                    if j + AHEAD < nk:
                        score(j + AHEAD)
                    P.act(R(pt[:, c0:512]), ps[:, c0:512], AF.Exp, scale=SC)
                    if jj >= 0:
                        if c0 > 0:
                            P.copy("dve", R(pt[:, 0:c0]), zt[:, 0:c0])
                        P.copy("dve", R(pt[64:128, c0:c0 + 64]), zt[64:128, 0:64])
                    P.mm(po[0:65, :], va_[:, j, :], R(pt), start=(j == 0), stop=(j == nk - 1))
                    if j == min(2, nk - 1):
                        while pending:
                            norm_tail(*pending.pop(0))
                cnt += nk
                pending.append((h, n, po))
        while pending:
            norm_tail(*pending.pop(0))
        P.pop()

    def stage3(l, dst):
        P.push()
        wbb = P.sb("wbb", [128, 4, 1024], F32R)
        wo = P.sb("wo", [128, 8, 1024], F32R)
        P.dma("sp", wbb.re("p c n -> p (c n)"), w3b[l])
        for kc in range(8):
            P.dma("sp", wo[:, kc, :], w3o[l, kc])
            P.tt("pool" if kc % 2 else "dve", wo[:, kc, :], wo[:, kc, :], G_t, ALU.mult)
        wba = P.sb("wba", [128, 4, 1024], F32R)
        P.dma("sp", wba.re("p c n -> p (c n)"), w3a[l])
        att = [P.sb("att", [128, 4, 512], F32R) for _ in range(2)]
        ont = [P.sb("ont", [128, 4, 512], F32R) for _ in range(2)]
        mat_ = [P.sb("mat3", [128, 512]) for _ in range(2)]
        gbt_ = [P.sb("gbt3", [128, 512]) for _ in range(2)]
        tmp = [P.sb("tmp3", [128, 512]) for _ in range(2)]
        mg = P.sb("mg", [128, 8, 512])
        xt = [P.sb("xt3", [128, D]) for _ in range(2)]
        xo = [P.sb("xo3", [128, D]) for _ in range(2)]
        for n in range(NT):
            cols = slice(n * 512, (n + 1) * 512)
            a_ = att[n % 2]
            for c in range(4):
                P.dma("sp", a_[:, c, :], atg[c % 2][(c // 2) * 128:(c // 2 + 1) * 128, cols].bc(F32R).sub(("atg", c % 2)))
            o_ = ont[n % 2]
            for gh in range(4):
                P.dma("sp", o_[:, gh, :], ong[gh % 2][(gh // 2) * 128:(gh // 2 + 1) * 128, cols].bc(F32R).sub(("ong", gh % 2)))
            for j in range(8):
                P.dma("sp", mat_[j % 2], ma_d[j * 128:(j + 1) * 128, cols].sub(("ma", j, n)))
                P.dma("sp", gbt_[j % 2], gb_d[j * 128:(j + 1) * 128, cols].sub(("gb", j, n)))
                bka = nb()
                for gh in range(4):
                    P.mm(bka, wba[:, gh, j * 128:(j + 1) * 128], o_[:, gh, :], start=(gh == 0), stop=(gh == 3))
                P.tt("dve", mat_[j % 2], bka, mat_[j % 2], ALU.mult)
                bank = nb()
                for kc in range(4):
                    P.mm(bank, wbb[:, kc, j * 128:(j + 1) * 128], a_[:, kc, :], start=(kc == 0), stop=(kc == 3))
                P.tt("dve", tmp[j % 2], bank, gbt_[j % 2], ALU.mult)
                P.tt("pool" if j % 2 == 0 else "dve", R(mg[:, j, :]), tmp[j % 2], mat_[j % 2], ALU.add)
            for s in range(4):
                r0 = n * 512 + s * 128
                bi = n * 4 + s
                P.dma("sp", xt[s % 2], xblk(l, bi))
                for half in range(2):
                    hs = slice(half * 512, (half + 1) * 512)
                    bank = nb()
                    for j in range(8):
                        P.mm(bank, R(mg[:, j, s * 128:(s + 1) * 128]), wo[:, j, hs], start=(j == 0), stop=(j == 7))
                    P.tt("dve", xo[s % 2][:, hs], bank, xt[s % 2][:, hs], ALU.add)
                P.dma("sp", dst[r0:r0 + 128, :].sub((dst.tok, bi)), xo[s % 2])
        P.pop()

    def stage4(l, src, dst, last):
        load_mod(l, 1)
        P.push()
        BIG = 1.0e4
        MT = 1024
        NB = MT // 128
        make_ring(12, 4)
        wrt = P.sb("wrt", [128, 8 * NEXP], F32R)
        rb = P.sb("rb", [128, NEXP])
        P.dma("sp", wrt, wrt_d)
        P.dma("sp", rb, rbias_d.bto([128, NEXP]))
        if last:
            FG = P.sb("FG", [128, D])
            P.dma("sp", FG, fing.bto([128, D]))
        hsel = P.sb("hsel", [128, 2], I32)
        P.dma("sp", hsel, hsel_d)
        xm = P.sb("xm", [128, NB, D])
        hh = [P.sb("hh4", [128, D]) for _ in range(2)]
        junk = P.sb("junk4", [128, D])
        ss = P.sb("ss4", [128, 1])
        h2T = P.sb("h2T", [128, 8, MT])
        yacc = P.sb("yacc", [128, NB, D])
        heT = P.sb("heT", [128, 4, MT])
        sil = [P.sb("sil", [128, 512]) for _ in range(2)]
        comb = P.sb("comb", [128, NB, NEXP])
        sm = lambda nm, w: P.sb(nm, [128, w])
        sc_, bi_, eq_, b2_, mk_, q1_, mk2_, q2_, ws_ = [sm("r%d" % i, 16) for i in range(9)]
        m1_, m2_, gs_, gsel_, pen_ = [sm("g%d" % i, 4) for i in range(5)]
        gm_, e1_, e2_, den_, rden_ = [sm("s%d" % i, 1) for i in range(5)]
        g3 = lambda v: v.re("p (g e) -> p g e", e=4)

        def route(blk, lg):
            P.act(sc_, lg, AF.Sigmoid)
            P.tt("dve", bi_, sc_, rb, ALU.add)
            P.reduce(m1_, g3(bi_), ALU.max)
            P.tt("dve", g3(eq_), g3(bi_), m1_.re("p (g o) -> p g o", o=1).bcast([128, 4, 4]), ALU.is_equal)
            P.stt(b2_, eq_, -BIG, bi_, ALU.mult, ALU.add)
            P.reduce(m2_, g3(b2_), ALU.max)
            P.tt("dve", gs_, m1_, m2_, ALU.add)
            P.reduce(gm_, gs_, ALU.max)
            P.ts("dve", gsel_, gs_, gm_, None, ALU.is_equal)
            P.ts("dve", pen_, gsel_, 1.0, BIG, ALU.subtract, ALU.mult)
            P.tt("dve", g3(mk_), g3(bi_), pen_.re("p (g o) -> p g o", o=1).bcast([128, 4, 4]), ALU.add)
            P.reduce(e1_, mk_, ALU.max)
            P.ts("dve", q1_, mk_, e1_, None, ALU.is_equal)
            P.stt(mk2_, q1_, -BIG, mk_, ALU.mult, ALU.add)
            P.reduce(e2_, mk2_, ALU.max)
            P.ts("dve", q2_, mk2_, e2_, None, ALU.is_equal)
            P.tt("dve", q1_, q1_, q2_, ALU.add)
            P.tt("dve", ws_, sc_, q1_, ALU.mult)
            P.reduce(den_, ws_, ALU.add)
            P.recip(rden_, den_)
            P.ts("dve", comb[:, blk, :], ws_, rden_, None, ALU.mult)

        def macro(m):
            gather_rows(xm.re("p b n -> p (b n)"), src.re("(s r) n -> s (r n)", r=NB), hsel[:, m:m + 1], T // NB - 1,
                        reads=[(src.tok, bi) for bi in range(T // 128)])
            for b in range(NB):
                norm_mod(xm[:, b, :], hh[b % 2], junk, ss)
                for half in range(2):
                    bank = nb()
                    for k4 in range(4):
                        kc = half * 4 + k4
                        P.tr(bank[:, k4 * 128:(k4 + 1) * 128], hh[b % 2][:, kc * 128:(kc + 1) * 128], ident)
                    P.copy("act" if half else "dve", R(h2T[:, half * 4:(half + 1) * 4, b * 128:(b + 1) * 128]),
                           bank.re("p (k t) -> p k t", t=128))
                for kc in range(8):
                    P.mm(pb[7][:, 0:NEXP], R(h2T[:, kc, b * 128:(b + 1) * 128]), wrt[:, kc * NEXP:(kc + 1) * NEXP],
                         start=(kc == 0), stop=(kc == 7))
                route(b, pb[7][:, 0:NEXP])
            for e in range(NEXP):
                for j in range(4):
                    sg_ = getw(w4[l, e, 2 * j], 1024)
                    su_ = getw(w4[l, e, 2 * j + 1], 1024)
                    for sub in range(MT // 512):
                        ts_ = slice(sub * 512, (sub + 1) * 512)
                        bg, bu = nb(), pb[3 + (j * 2 + sub) % 2]
                        for kc in range(8):
                            P.mm(bg, sg_[:, kc * 128:(kc + 1) * 128], R(h2T[:, kc, ts_]), start=(kc == 0), stop=(kc == 7))
                        for kc in range(8):
                            P.mm(bu, su_[:, kc * 128:(kc + 1) * 128], R(h2T[:, kc, ts_]), start=(kc == 0), stop=(kc == 7))
                        s_ = sil[sub % 2]
                        P.act(s_, bg, AF.Silu)
                        P.tt("dve", R(heT[:, j, ts_]), s_, bu, ALU.mult)
                sd = [getw(w4[l, e, 8 + j], 1024) for j in range(4)]
                for b in range(NB):
                    for half in range(2):
                        hs = slice(half * 512, (half + 1) * 512)
                        bank = pb[5 + (b * 2 + half) % 2]
                        for j in range(4):
                            P.mm(bank, R(heT[:, j, b * 128:(b + 1) * 128]), sd[j][:, hs], start=(j == 0), stop=(j == 3))
                        if e == 0:
                            P.ts("dve", yacc[:, b, hs], bank, comb[:, b, 0:1], None, ALU.mult)
                        else:
                            P.stt(yacc[:, b, hs], bank, comb[:, b, e:e + 1], yacc[:, b, hs], ALU.mult, ALU.add)
            dview = dst.re("(m p r) n -> m p r n", p=128, r=NB) if last else None
            for b in range(NB):
                P.tt("dve", yacc[:, b, :], yacc[:, b, :], G_t, ALU.mult)
                P.tt("dve", hh[b % 2], yacc[:, b, :], xm[:, b, :], ALU.add)
                dtk = (dst.tok, m, b)
                if not last:
                    for hc in range(2):
                        cv = xh_c[2 * m + hc].re("(p r) n -> p r n", r=NB)
                        P.dma("sp", cv[:, b, :].sub(("xh", m, b, hc)), hh[b % 2][hc * 64:(hc + 1) * 64, :])
                else:
                    P.act(junk, hh[b % 2], AF.Square, accum=ss)
                    P.act(ss, ss, AF.Ln, scale=1.0 / D, bias=eps_t)
                    P.act(ss, ss, AF.Exp, scale=-0.5)
                    P.stt(yacc[:, b, :], hh[b % 2], ss, FG, ALU.mult, ALU.mult)
                    P.dma("sp", dview[m][:, b, :].sub(dtk), yacc[:, b, :])

        NM = TH // MT

        def all_macros():
            for m in range(NM):
                macro(m)
                if not last:
                    for c in range(m * MT // CR, (m + 1) * MT // CR):
                        rd = [("xh", m, b, c % 2) for b in range(NB)]
                        wr = [("xb", (h * TH + c * CR) // 128 + q) for h in range(2) for q in range(CR // 128)]
                        pool_dma(lambda eng, c=c: eng.collective_compute(
                            "AllGather", ALU.bypass, replica_groups=[[0, 1], [2, 3], [4, 5], [6, 7]],
                            ins=[xh_c[c].ap], outs=[xg[c].ap]), rd, wr, coll=True)

        plan_weights(all_macros)
        all_macros()
        P.pop()

    for l in range(nlayers):
        stage1(l)
        if upto == "s1":
            break
        stage2(l)
        if upto == "s2":
            break
        stage3(l, xa)
        if upto == "s3":
            break
        last = (l == nlayers - 1)
        stage4(l, xa, out_d, last)
    P.barrier()
    P.emit()
    P.close()
    return nc


def core_inputs(inp, W, b, T, half=0):
    f = lambda a: np.asarray(a, dtype=np.float32)
    d = dict(W)
    d["w1"] = W["w1"][half]
    d["lbl"] = W["lbl"][half]
    d["hgn"] = W["hgn"][half]
    d["x"] = np.ascontiguousarray(f(inp["x"])[b, :T])
    d["cvec"] = np.ascontiguousarray(f(inp["c"])[b].reshape(8, 128).T)
    d["pos"] = np.ascontiguousarray(np.asarray(inp["positions"], dtype=np.int32)[b:b + 1, :T])
    sup = (T // 2) // 8
    d["hsel"] = np.ascontiguousarray((half * sup + np.arange(2)[None, :] * 128 + np.arange(128)[:, None]).astype(np.int32))
    return d


def kernel(**inputs):
    T = 4096
    W = prep_weights(inputs)
    nc = build(T)
    in_maps = [core_inputs(inputs, W, i // 2, T, half=i % 2) for i in range(8)]
    res = run_bass_kernel_spmd(nc, in_maps, core_ids=list(range(8)))
    out = np.stack([np.concatenate([np.asarray(res.results[2 * b + h]["out"], dtype=np.float32) for h in range(2)], axis=0)
                    for b in range(4)], axis=0)
    return out
```
